# Optimizing a Trainium2 kernel written in Bass

```python
import jax, jax.numpy as jnp
from jax import lax
import numpy as np

D_MODEL = 1024
BATCH = 4
SEQ = 8192
DEPTH = 2

HEAD_DIM = 64
NSA_HEADS = 8
NSA_KV_HEADS = 2
CMP_LEN = 32
CMP_STRIDE = 16
SEL_LEN = 64
SEL_TOPK = 16
NSA_WINDOW = 512
SWA_HEADS = 8
SWA_KV_HEADS = 2
SWA_WINDOW = 128
Q_BLOCK = 128
MLSTM_HEADS = 4
MLSTM_QK_DIM = 128
MLSTM_V_DIM = 256
MLSTM_CHUNK = 64
CONV_WIDTH = 4
FFN_HIDDEN = -(-8 * D_MODEL // (3 * 256)) * 256
RMS_EPS = 1e-6
NEG_INF = -1e30
FORCE_SCORE = 1e9

NSA_REP = NSA_HEADS // NSA_KV_HEADS
SWA_REP = SWA_HEADS // SWA_KV_HEADS
AB_SPLITS = (NSA_HEADS * HEAD_DIM,) + (NSA_KV_HEADS * HEAD_DIM,) * 6 + (NSA_HEADS * 3, SWA_HEADS * HEAD_DIM, SWA_KV_HEADS * HEAD_DIM, SWA_KV_HEADS * HEAD_DIM)
AB_IN = sum(AB_SPLITS)
AB_MIX = (NSA_HEADS + SWA_HEADS) * HEAD_DIM
C_SPLITS = (2 * MLSTM_HEADS * MLSTM_QK_DIM, MLSTM_HEADS * MLSTM_V_DIM, MLSTM_HEADS * MLSTM_V_DIM, MLSTM_HEADS, MLSTM_HEADS)
C_IN = sum(C_SPLITS)
C_MIX = MLSTM_HEADS * MLSTM_V_DIM

kernel_name = 'nsa_swasink_mlstm_hybrid'


def _split(z, sizes):
    idx = np.cumsum(np.array(sizes))[:-1].tolist()
    return jnp.split(z, idx, axis=-1)


def rmsnorm(x, g):
    xf = x.astype(jnp.float32)
    y = xf * lax.rsqrt(jnp.mean(xf * xf, axis=-1, keepdims=True) + RMS_EPS)
    return (y * g.astype(jnp.float32)).astype(x.dtype)


def swiglu(x, wg, wu, wd):
    return (jax.nn.silu(x @ wg) * (x @ wu)) @ wd


def masked_softmax(logits, mask):
    p = jax.nn.softmax(jnp.where(mask, logits, NEG_INF), axis=-1)
    return jnp.where(mask, p, 0.0)


def selection_map(seq):
    nc = (seq - CMP_LEN) // CMP_STRIDE + 1
    ns = seq // SEL_LEN
    cs = np.arange(nc, dtype=np.int32)[:, None] * CMP_STRIDE
    ss = np.arange(ns, dtype=np.int32)[None, :] * SEL_LEN
    return ((cs < ss + SEL_LEN) & (cs + CMP_LEN > ss)).astype(np.float32)


def compress_kv(kv, pos, w1, w2):
    b, t = kv.shape[:2]
    nc = (t - CMP_LEN) // CMP_STRIDE + 1
    idx = np.arange(nc, dtype=np.int32)[:, None] * CMP_STRIDE + np.arange(CMP_LEN, dtype=np.int32)[None, :]
    blk = kv[:, idx] + pos[None, None, :, None, :]
    blk = blk.transpose(0, 3, 1, 2, 4).reshape(b, kv.shape[2], nc, CMP_LEN * HEAD_DIM)
    return jax.nn.silu(blk @ w1) @ w2


def nsa_attention(q, kc, vc, ksel, vsel, kwin, vwin, gates):
    b, t = q.shape[:2]
    nc = kc.shape[2]
    nb = t // Q_BLOCK
    ns = t // SEL_LEN
    topk = min(SEL_TOPK, ns)
    scale = HEAD_DIM ** -0.5
    sel_map = jnp.asarray(selection_map(t))
    cmp_end = jnp.asarray(np.arange(nc, dtype=np.int32) * CMP_STRIDE + CMP_LEN - 1)
    ksb = ksel.reshape(b, ns, SEL_LEN, NSA_KV_HEADS, HEAD_DIM).transpose(0, 3, 1, 2, 4)
    vsb = vsel.reshape(b, ns, SEL_LEN, NSA_KV_HEADS, HEAD_DIM).transpose(0, 3, 1, 2, 4)
    pad = ((0, 0), (NSA_WINDOW, 0), (0, 0), (0, 0))
    kwp = jnp.pad(kwin, pad)
    vwp = jnp.pad(vwin, pad)
    span = NSA_WINDOW + Q_BLOCK
    gather = jax.vmap(jax.vmap(lambda kb, ix: kb[ix]))
    blocks = jnp.arange(ns, dtype=jnp.int32)

    def block(n):
        start = n * Q_BLOCK
        qb = lax.dynamic_slice_in_dim(q, start, Q_BLOCK, axis=1).transpose(0, 2, 3, 1, 4)
        gb = lax.dynamic_slice_in_dim(gates, start, Q_BLOCK, axis=1).transpose(0, 2, 3, 1, 4)
        tq = start + jnp.arange(Q_BLOCK, dtype=jnp.int32)
        s = jnp.einsum('bgrtd,bgcd->bgrtc', qb, kc).astype(jnp.float32) * scale
        p_c = masked_softmax(s, cmp_end[None, :] <= tq[:, None])
        o_c = jnp.einsum('bgrtc,bgcd->bgrtd', p_c.astype(vc.dtype), vc)
        imp = jnp.einsum('bgrtc,cs->bgts', p_c, sel_map)
        cur = tq // SEL_LEN
        valid = blocks[None, :] * SEL_LEN <= tq[:, None]
        forced = (blocks[None, :] == 0) | (blocks[None, :] == cur[:, None]) | (blocks[None, :] == cur[:, None] - 1)
        score = jnp.where(forced, FORCE_SCORE, jnp.where(valid, imp, NEG_INF))
        top_s, top_i = lax.top_k(score, topk)
        kg = gather(ksb, top_i)
        vg = gather(vsb, top_i)
        kpos = top_i[..., None] * SEL_LEN + jnp.arange(SEL_LEN, dtype=jnp.int32)
        m_s = (top_s > NEG_INF * 0.5)[..., None] & (kpos <= tq[:, None, None])
        m_s = m_s.reshape(b, NSA_KV_HEADS, 1, Q_BLOCK, topk * SEL_LEN)
        s = jnp.einsum('bgrtd,bgtkjd->bgrtkj', qb, kg).astype(jnp.float32) * scale
        p_s = masked_softmax(s.reshape(b, NSA_KV_HEADS, NSA_REP, Q_BLOCK, topk * SEL_LEN), m_s)
        o_s = jnp.einsum('bgrtm,bgtmd->bgrtd', p_s.astype(vg.dtype), vg.reshape(b, NSA_KV_HEADS, Q_BLOCK, topk * SEL_LEN, HEAD_DIM))
        kw = lax.dynamic_slice_in_dim(kwp, start, span, axis=1)
        vw = lax.dynamic_slice_in_dim(vwp, start, span, axis=1)
        sp = start - NSA_WINDOW + jnp.arange(span, dtype=jnp.int32)
        m_w = (sp[None, :] >= 0) & (sp[None, :] <= tq[:, None]) & (tq[:, None] - sp[None, :] < NSA_WINDOW)
        s = jnp.einsum('bgrtd,bsgd->bgrts', qb, kw).astype(jnp.float32) * scale
        p_w = masked_softmax(s, m_w)
        o_w = jnp.einsum('bgrts,bsgd->bgrtd', p_w.astype(vw.dtype), vw)
        o = gb[..., 0:1] * o_c + gb[..., 1:2] * o_s + gb[..., 2:3] * o_w
        return o.transpose(0, 3, 1, 2, 4).reshape(b, Q_BLOCK, NSA_HEADS * HEAD_DIM)

    out = lax.map(block, jnp.arange(nb, dtype=jnp.int32))
    return out.transpose(1, 0, 2, 3).reshape(b, t, NSA_HEADS * HEAD_DIM)


def swa_sink_attention(q, k, v, sinks):
    b, t = q.shape[:2]
    nb = t // Q_BLOCK
    span = SWA_WINDOW + Q_BLOCK
    scale = HEAD_DIM ** -0.5
    pad = ((0, 0), (SWA_WINDOW, 0), (0, 0), (0, 0))
    kp = jnp.pad(k, pad)
    vp = jnp.pad(v, pad)
    sink = sinks.astype(jnp.float32).reshape(1, SWA_KV_HEADS, SWA_REP, 1, 1)

    def block(n):
        start = n * Q_BLOCK
        qb = lax.dynamic_slice_in_dim(q, start, Q_BLOCK, axis=1).transpose(0, 2, 3, 1, 4)
        kb = lax.dynamic_slice_in_dim(kp, start, span, axis=1)
        vb = lax.dynamic_slice_in_dim(vp, start, span, axis=1)
        tq = start + jnp.arange(Q_BLOCK, dtype=jnp.int32)
        sp = start - SWA_WINDOW + jnp.arange(span, dtype=jnp.int32)
        mask = (sp[None, :] >= 0) & (sp[None, :] <= tq[:, None]) & (tq[:, None] - sp[None, :] < SWA_WINDOW)
        logits = jnp.where(mask, jnp.einsum('bgrtd,bsgd->bgrts', qb, kb).astype(jnp.float32) * scale, NEG_INF)
        mx = jnp.maximum(logits.max(axis=-1, keepdims=True), sink)
        e = jnp.exp(logits - mx)
        p = e / (e.sum(axis=-1, keepdims=True) + jnp.exp(sink - mx))
        o = jnp.einsum('bgrts,bsgd->bgrtd', p.astype(vb.dtype), vb)
        return o.transpose(0, 3, 1, 2, 4).reshape(b, Q_BLOCK, SWA_HEADS * HEAD_DIM)

    out = lax.map(block, jnp.arange(nb, dtype=jnp.int32))
    return out.transpose(1, 0, 2, 3).reshape(b, t, SWA_HEADS * HEAD_DIM)


def nsa_swa_mixer(h, w_in, gate_bias, pos_k, pos_v, ck_w1, ck_w2, cv_w1, cv_w2, sinks, w_out):
    b, t, _ = h.shape
    qa, kc, vc, ks, vs, kw, vw, g, qb, kb, vb = _split(h @ w_in, AB_SPLITS)
    kv = lambda a: a.reshape(b, t, NSA_KV_HEADS, HEAD_DIM)
    qa = qa.reshape(b, t, NSA_KV_HEADS, NSA_REP, HEAD_DIM)
    kc = compress_kv(kv(kc), pos_k, ck_w1, ck_w2)
    vc = compress_kv(kv(vc), pos_v, cv_w1, cv_w2)
    gates = jax.nn.sigmoid(g + gate_bias).reshape(b, t, NSA_KV_HEADS, NSA_REP, 3)
    o_a = nsa_attention(qa, kc, vc, kv(ks), kv(vs), kv(kw), kv(vw), gates)
    qb = qb.reshape(b, t, SWA_KV_HEADS, SWA_REP, HEAD_DIM)
    kb = kb.reshape(b, t, SWA_KV_HEADS, HEAD_DIM)
    vb = vb.reshape(b, t, SWA_KV_HEADS, HEAD_DIM)
    o_b = swa_sink_attention(qb, kb, vb, sinks)
    return jnp.concatenate([o_a, o_b], axis=-1) @ w_out


def causal_depthwise_conv(x, w, bias):
    ch = x.shape[-1]
    y = lax.conv_general_dilated(x, w[:, None, :].astype(x.dtype), window_strides=(1,), padding=((CONV_WIDTH - 1, 0),), dimension_numbers=('NWC', 'WIO', 'NWC'), feature_group_count=ch)
    return y + bias


def mlstm_chunkwise(q, k, v, logi, logf):
    b, nh, t, dk = q.shape
    dv = v.shape[-1]
    L = MLSTM_CHUNK
    nc = t // L

    def chunks(a):
        return jnp.moveaxis(a.reshape(a.shape[:2] + (nc, L) + a.shape[3:]), 2, 0)

    causal = jnp.tril(jnp.ones((L, L), dtype=bool))

    def step(carry, inp):
        C, n, m = carry
        qc, kc, vc, ic, fc = inp
        bcum = jnp.cumsum(fc, axis=-1)
        a = bcum + m[..., None]
        D = jnp.where(causal, bcum[..., :, None] - bcum[..., None, :] + ic[..., None, :], -jnp.inf)
        mt = jnp.maximum(a, D.max(axis=-1))
        w_inter = jnp.exp(a - mt)
        S = jnp.einsum('bhtd,bhsd->bhts', qc, kc) * jnp.exp(D - mt[..., None])
        num = w_inter[..., None] * jnp.einsum('bhtd,bhdv->bhtv', qc, C) + jnp.einsum('bhts,bhsv->bhtv', S, vc)
        den = w_inter * jnp.einsum('bhtd,bhd->bht', qc, n) + S.sum(axis=-1)
        hc = num / jnp.maximum(jnp.abs(den), jnp.exp(-mt))[..., None]
        bl = bcum[..., -1]
        wl = bl[..., None] - bcum + ic
        m_new = jnp.maximum(bl + m, wl.max(axis=-1))
        decay = jnp.exp(bl + m - m_new)
        w = jnp.exp(wl - m_new[..., None])
        C_new = decay[..., None, None] * C + jnp.einsum('bhs,bhsd,bhsv->bhdv', w, kc, vc)
        n_new = decay[..., None] * n + jnp.einsum('bhs,bhsd->bhd', w, kc)
        return (C_new, n_new, m_new), hc

    init = (jnp.zeros((b, nh, dk, dv), jnp.float32), jnp.zeros((b, nh, dk), jnp.float32), jnp.zeros((b, nh), jnp.float32))
    _, hs = lax.scan(step, init, (chunks(q), chunks(k), chunks(v), chunks(logi), chunks(logf)))
    return jnp.moveaxis(hs, 0, 2).reshape(b, nh, t, dv)


def mlstm_mixer(h, w_in, conv_w, conv_b, igate_bias, fgate_bias, w_out):
    b, t, _ = h.shape
    qk, v, og, ig, fg = _split(h @ w_in, C_SPLITS)
    qk = jax.nn.silu(causal_depthwise_conv(qk, conv_w, conv_b))
    q, k = jnp.split(qk, 2, axis=-1)

    def heads(a, d):
        return a.reshape(b, t, MLSTM_HEADS, d).transpose(0, 2, 1, 3).astype(jnp.float32)

    q = heads(q, MLSTM_QK_DIM)
    k = heads(k, MLSTM_QK_DIM) * (MLSTM_QK_DIM ** -0.5)
    v = heads(v, MLSTM_V_DIM)
    logi = (ig + igate_bias).astype(jnp.float32).transpose(0, 2, 1)
    logf = jax.nn.log_sigmoid((fg + fgate_bias).astype(jnp.float32)).transpose(0, 2, 1)
    hh = mlstm_chunkwise(q, k, v, logi, logf)
    hh = hh.transpose(0, 2, 1, 3).reshape(b, t, C_MIX).astype(h.dtype) * jax.nn.sigmoid(og)
    return hh @ w_out


def setup_inputs(seed: int = 0) -> dict:
    key = jax.random.key(seed)
    ks = jax.random.split(key, 24)
    ne = (DEPTH + 1) // 2
    no = DEPTH // 2

    def nrm(k, shape, scale):
        return jax.random.normal(k, shape, jnp.float32) * scale

    qk_ch = 2 * MLSTM_HEADS * MLSTM_QK_DIM
    return {
        'x': nrm(ks[0], (BATCH, SEQ, D_MODEL), 1.0),
        'norm_mix': 1.0 + nrm(ks[1], (DEPTH, D_MODEL), 0.02),
        'norm_ffn': 1.0 + nrm(ks[2], (DEPTH, D_MODEL), 0.02),
        'ffn_w_gate': nrm(ks[3], (DEPTH, D_MODEL, FFN_HIDDEN), D_MODEL ** -0.5),
        'ffn_w_up': nrm(ks[4], (DEPTH, D_MODEL, FFN_HIDDEN), D_MODEL ** -0.5),
        'ffn_w_down': nrm(ks[5], (DEPTH, FFN_HIDDEN, D_MODEL), FFN_HIDDEN ** -0.5),
        'ab_w_in': nrm(ks[6], (ne, D_MODEL, AB_IN), D_MODEL ** -0.5),
        'ab_gate_bias': nrm(ks[7], (ne, NSA_HEADS * 3), 0.1),
        'nsa_pos_k': nrm(ks[8], (ne, CMP_LEN, HEAD_DIM), 0.1),
        'nsa_pos_v': nrm(ks[9], (ne, CMP_LEN, HEAD_DIM), 0.1),
        'nsa_cmp_k_w1': nrm(ks[10], (ne, CMP_LEN * HEAD_DIM, HEAD_DIM), (CMP_LEN * HEAD_DIM) ** -0.5),
        'nsa_cmp_k_w2': nrm(ks[11], (ne, HEAD_DIM, HEAD_DIM), HEAD_DIM ** -0.5),
        'nsa_cmp_v_w1': nrm(ks[12], (ne, CMP_LEN * HEAD_DIM, HEAD_DIM), (CMP_LEN * HEAD_DIM) ** -0.5),
        'nsa_cmp_v_w2': nrm(ks[13], (ne, HEAD_DIM, HEAD_DIM), HEAD_DIM ** -0.5),
        'swa_sinks': nrm(ks[14], (ne, SWA_HEADS), 0.5),
        'ab_w_out': nrm(ks[15], (ne, AB_MIX, D_MODEL), AB_MIX ** -0.5),
        'c_w_in': nrm(ks[16], (no, D_MODEL, C_IN), D_MODEL ** -0.5),
        'c_conv_w': nrm(ks[17], (no, CONV_WIDTH, qk_ch), CONV_WIDTH ** -0.5),
        'c_conv_b': nrm(ks[18], (no, qk_ch), 0.02),
        'c_igate_bias': nrm(ks[19], (no, MLSTM_HEADS), 0.1),
        'c_fgate_bias': jnp.linspace(3.0, 6.0, MLSTM_HEADS, dtype=jnp.float32)[None, :] + nrm(ks[20], (no, MLSTM_HEADS), 0.1),
        'c_w_out': nrm(ks[21], (no, C_MIX, D_MODEL), C_MIX ** -0.5),
        'final_norm': 1.0 + nrm(ks[22], (D_MODEL,), 0.02),
    }


def reference(x, norm_mix, norm_ffn, ffn_w_gate, ffn_w_up, ffn_w_down, ab_w_in, ab_gate_bias, nsa_pos_k, nsa_pos_v, nsa_cmp_k_w1, nsa_cmp_k_w2, nsa_cmp_v_w1, nsa_cmp_v_w2, swa_sinks, ab_w_out, c_w_in, c_conv_w, c_conv_b, c_igate_bias, c_fgate_bias, c_w_out, final_norm):
    h = x
    for layer in range(DEPTH):
        j = layer // 2
        hn = rmsnorm(h, norm_mix[layer])
        if layer % 2 == 0:
            h = h + nsa_swa_mixer(hn, ab_w_in[j], ab_gate_bias[j], nsa_pos_k[j], nsa_pos_v[j], nsa_cmp_k_w1[j], nsa_cmp_k_w2[j], nsa_cmp_v_w1[j], nsa_cmp_v_w2[j], swa_sinks[j], ab_w_out[j])
        else:
            h = h + mlstm_mixer(hn, c_w_in[j], c_conv_w[j], c_conv_b[j], c_igate_bias[j], c_fgate_bias[j], c_w_out[j])
        h = h + swiglu(rmsnorm(h, norm_ffn[layer]), ffn_w_gate[layer], ffn_w_up[layer], ffn_w_down[layer])
    return rmsnorm(h, final_norm)
```

```python
import numpy as np
import ml_dtypes
from contextlib import ExitStack
import concourse.bass as bass
import concourse.mybir as mybir
from concourse.bass_utils import run_bass_kernel_spmd

F32 = mybir.dt.float32
BF16 = mybir.dt.bfloat16
AF = mybir.ActivationFunctionType
ALU = mybir.AluOpType
AX = mybir.AxisListType
NPBF = ml_dtypes.bfloat16

D = 1024
SEQ = 8192
BATCH = 4
NCORES = 8
FFN = 2816
EPS = 1e-6


class V:
    __slots__ = ("tile", "ap")

    def __init__(self, tile, ap):
        self.tile = tile
        self.ap = ap


class T:
    def __init__(self, prog, handle, name):
        self.p = prog
        self.h = handle
        self.name = name
        self.lw = None
        self.rd = []
        self.dsem = None
        self.dcnt = 0
        self.track = True
        self.onchip = True
        self.psum = False

    def __getitem__(self, idx):
        return V(self, self.h[idx])

    def v(self, ap):
        return V(self, ap)

    def all(self):
        return V(self, self.h[:])


ENG = ("pe", "act", "dve", "pool", "sp")


class Prog:
    def __init__(self):
        self.nc = bass.Bass("TRN2", target_bir_lowering=False)
        self.es = ExitStack()
        self.ops = {e: [] for e in ENG}
        self.cnt = {e: 0 for e in ENG}
        self.waited = {e: {} for e in ENG}
        self.sems = {}
        self.nsem = 0
        for e in ENG:
            self.sems[e] = self.es.enter_context(self.nc.semaphore("s_" + e))
        self.out_tokens = []
        self.uid = 0

    def dram(self, name, shape, dt, kind):
        h = self.nc.dram_tensor(name, list(shape), dt, kind=kind)
        t = T(self, h, name)
        t.onchip = False
        t.track = False
        return t

    def sb(self, name, shape, dt):
        h = self.es.enter_context(self.nc.sbuf_tensor(name, list(shape), dt))
        return T(self, h, name)

    def ps(self, name, shape, dt):
        h = self.es.enter_context(self.nc.psum_tensor(name, list(shape), dt))
        t = T(self, h, name)
        t.psum = True
        return t

    def _dsem(self, t):
        if t.dsem is None:
            self.nsem += 1
            t.dsem = self.es.enter_context(self.nc.semaphore("d%d_%s" % (self.nsem, t.name)))
            self.sems[id(t)] = t.dsem
        return t.dsem

    def op(self, eng, fn, writes, reads, dma=False, is_out=False, semtile=None):
        deps = {}

        def add(tok):
            if tok is None:
                return
            k, v = tok
            if deps.get(k, 0) < v:
                deps[k] = v

        wt = []
        for w in writes:
            if w.tile not in wt:
                wt.append(w.tile)
        rt = []
        for r in reads:
            if r.tile not in rt and r.tile not in wt:
                rt.append(r.tile)
        for t in rt:
            if t.track:
                add(t.lw)
                if t.psum:
                    for r in t.rd:
                        if r[0] != eng:
                            add(r)
        for t in wt:
            if t.track:
                add(t.lw)
                for r in t.rd:
                    add(r)
        waits = []
        for k, v in deps.items():
            if k == eng and eng == "pe" and not dma:
                continue
            if self.waited[eng].get(k, 0) >= v:
                continue
            self.waited[eng][k] = v
            waits.append((self.sems[k], v))
        if dma:
            n = dma if isinstance(dma, int) and dma is not True else 1
            sem = self._dsem(semtile)
            semtile.dcnt += 16 * n
            tok = (id(semtile), semtile.dcnt)
            inc = (sem, 16)
        else:
            self.cnt[eng] += 1
            tok = (eng, self.cnt[eng])
            inc = (self.sems[eng], 1)
        self.ops[eng].append((waits, fn, inc))
        for t in wt:
            if t.track:
                t.lw = tok
                t.rd = []
        for t in rt:
            if t.track:
                t.rd.append(tok)
        if is_out:
            self.out_tokens.append(tok)
        return tok

    def dma(self, out, in_, eng="sp", is_out=False):
        outs = out if isinstance(out, list) else [out]
        ins = in_ if isinstance(in_, list) else [in_]
        pairs = [(o.ap, i.ap) for o, i in zip(outs, ins)]
        semtile = outs[0].tile if outs[0].tile.onchip else ins[0].tile
        n = len(pairs)

        def fn(e):
            return [e.dma_start(out=o, in_=i) for o, i in pairs]
        return self.op(eng, fn, outs, ins, dma=n, is_out=is_out, semtile=semtile)

    def mm(self, out, lhsT, rhs, start=True, stop=True, skip=False):
        o, l, r = out.ap, lhsT.ap, rhs.ap
        if skip:
            return self.op("pe", lambda e: e.matmul(o, l, r, start=start, stop=stop, skip_group_check=True),
                           [out], [lhsT, rhs])
        return self.op("pe", lambda e: e.matmul(o, l, r, start=start, stop=stop), [out], [lhsT, rhs])

    def tr(self, out, in_, ident):
        o, i, d = out.ap, in_.ap, ident.ap
        return self.op("pe", lambda e: e.transpose(o, i, d), [out], [in_, ident])

    def act(self, out, in_, func, bias=None, scale=None, accum=None, eng="act"):
        o, i = out.ap, in_.ap
        kw = {}
        rd = [in_]
        wr = [out]
        if bias is not None:
            if isinstance(bias, V):
                kw["bias"] = bias.ap
                rd.append(bias)
            else:
                kw["bias"] = bias
        if scale is not None:
            if isinstance(scale, V):
                kw["scale"] = scale.ap
                rd.append(scale)
            else:
                kw["scale"] = scale
        if accum is not None:
            kw["accum_out"] = accum.ap
            wr.append(accum)
        return self.op("act", lambda e: e.activation(o, i, func, **kw), wr, rd)

    def tt(self, out, a, b, op, eng="dve"):
        o, x, y = out.ap, a.ap, b.ap
        return self.op(eng, lambda e: e.tensor_tensor(o, x, y, op), [out], [a, b])

    def ts(self, out, a, s1, s2, op0, op1=None, eng="dve", accum=None):
        o, x = out.ap, a.ap
        rd = [a]
        wr = [out]
        if isinstance(s1, V):
            rd.append(s1)
            s1 = s1.ap
        if isinstance(s2, V):
            rd.append(s2)
            s2 = s2.ap
        kw = {}
        if op1 is not None:
            kw["op1"] = op1
        if accum is not None:
            kw["accum_out"] = accum.ap
            wr.append(accum)
        return self.op(eng, lambda e: e.tensor_scalar(o, x, s1, s2, op0, **kw), wr, rd)

    def stt(self, out, a, s, b, op0, op1, eng="dve"):
        o, x, y = out.ap, a.ap, b.ap
        rd = [a, b]
        if isinstance(s, V):
            rd.append(s)
            s = s.ap
        return self.op(eng, lambda e: e.scalar_tensor_tensor(o, x, s, y, op0, op1), [out], rd)

    def copy(self, out, in_, eng="dve"):
        o, i = out.ap, in_.ap
        if eng == "act":
            return self.op("act", lambda e: e.copy(o, i), [out], [in_])
        return self.op(eng, lambda e: e.tensor_copy(o, i), [out], [in_])

    def memset(self, out, val, eng="dve"):
        o = out.ap
        return self.op(eng, lambda e: e.memset(o, val), [out], [])

    def recip(self, out, in_):
        o, i = out.ap, in_.ap
        return self.op("dve", lambda e: e.reciprocal(o, i), [out], [in_])

    def finish(self):
        nc = self.nc
        fin = {}
        for k, v in self.out_tokens:
            fin[k] = max(fin.get(k, 0), v)
        ops = self.ops
        sems = self.sems
        engobj = {"pe": "tensor", "act": "scalar", "dve": "vector", "pool": "gpsimd", "sp": "sync"}
        with nc.Block() as block:
            def mk(ename):
                def body(e):
                    for waits, fn, inc in ops[ename]:
                        for s, v in waits:
                            e.wait_ge(s, v)
                        r = fn(e)
                        if isinstance(r, list):
                            for x in r:
                                x.then_inc(inc[0], inc[1])
                        else:
                            r.then_inc(inc[0], inc[1])
                    if ename == "sp":
                        for k, v in fin.items():
                            e.wait_ge(sems[k], v)
                return body
            for ename in ENG:
                if ops[ename] or ename == "sp":
                    getattr(block, engobj[ename])(mk(ename))
        self.es.close()
        return nc


def run_prog(prog, in_maps):
    nc = prog.finish()
    res = run_bass_kernel_spmd(nc, in_maps, core_ids=list(range(len(in_maps))))
    return res.results


def load_weight_bf16(p, wdram, wb, K, N, stage, col0=0, engs=("dve", "act")):
    SW = stage[0].h.shape[1]
    i = 0
    for kc in range(K // 128):
        for c0 in range(0, N, SW):
            w = min(SW, N - c0)
            st = stage[i % len(stage)]
            p.dma(st[:, 0:w], wdram[kc * 128:(kc + 1) * 128, c0:c0 + w])
            p.copy(wb[:, kc, col0 + c0:col0 + c0 + w], st[:, 0:w], eng=engs[i % len(engs)])
            i += 1


def rms_to_fm(p, xt, hnT, tslot, g_rep, ident, scr, xs, pT, ssq, rstd):
    p.act(scr[:, :], xt[:, :], AF.Square, accum=ssq[:, 0:1])
    p.act(rstd[:, 0:1], ssq[:, 0:1], AF.Sqrt, bias=EPS, scale=1.0 / D)
    p.recip(rstd[:, 0:1], rstd[:, 0:1])
    p.ts(xs[:, :], xt[:, :], rstd[:, 0:1], None, ALU.mult)
    for c in range(8):
        p.tr(pT[:, c * 128:(c + 1) * 128], xs[:, c * 128:(c + 1) * 128], ident[:, :])
    p.tt(hnT[:, 0:8, tslot * 128:(tslot + 1) * 128],
         pT.v(pT.h[:, :].rearrange("p (c t) -> p c t", c=8)),
         g_rep.v(g_rep.h[:, :].rearrange("p (c t) -> p c t", c=8)), ALU.mult)


def build_proj(NT, CF, fm_dt, tm_segs, G=512):
    p = Prog()
    CT = sum(w for _, w, _ in tm_segs)
    N = CF + CT
    h = p.dram("h", [NT, D], F32, "ExternalInput")
    W = p.dram("W", [D, N], F32, "ExternalInput")
    g_rep_d = p.dram("g_rep", [128, 1024], F32, "ExternalInput")
    ident_d = p.dram("ident", [128, 128], BF16, "ExternalInput")
    zfm = p.dram("zfm", [max(CF, 128), NT], fm_dt, "ExternalOutput")
    ztm = [p.dram("ztm_" + nm, [NT, w], dt, "ExternalOutput") for nm, w, dt in tm_segs]

    wb = p.sb("wb", [128, 8, N], BF16)
    stage = [p.sb("stage%d" % i, [128, 1024], F32) for i in range(2)]
    g_rep = p.sb("g_rep_sb", [128, 1024], F32)
    ident = p.sb("ident_sb", [128, 128], BF16)
    xt = [p.sb("xt%d" % i, [128, D], F32) for i in range(2)]
    scr = p.sb("scr", [128, D], BF16)
    xs = [p.sb("xs%d" % i, [128, D], BF16) for i in range(2)]
    ssq = [p.sb("ssq%d" % i, [128, 1], F32) for i in range(2)]
    rstd = [p.sb("rstd%d" % i, [128, 1], F32) for i in range(2)]
    hnT = [p.sb("hnT%d" % i, [128, 8, G], BF16) for i in range(2)]
    ofm = [p.sb("ofm%d" % i, [128, G], fm_dt) for i in range(3)]
    otm = [[p.sb("otm_%s%d" % (nm, i), [128, w], dt) for i in range(2)] for nm, w, dt in tm_segs]
    pT = [p.ps("pT%d" % i, [128, 1024], BF16) for i in range(2)]
    pm = [p.ps("pm%d" % i, [128, 512], F32) for i in range(4)]

    p.dma(g_rep[:, :], g_rep_d[:, :])
    p.dma(ident[:, :], ident_d[:, :])
    load_weight_bf16(p, W, wb, D, N, stage)

    TPG = G // 128
    it = 0
    k_pm = 0
    k_of = 0
    for gi in range(NT // G):
        hb = hnT[gi % 2]
        for ti in range(TPG):
            t0 = gi * G + ti * 128
            b = it % 2
            p.dma(xt[b][:, :], h[t0:t0 + 128, :])
            rms_to_fm(p, xt[b], hb, ti, g_rep, ident, scr, xs[b], pT[b], ssq[b], rstd[b])
            it += 1
        for m in range(CF // 128):
            ps_ = pm[k_pm % 4]
            k_pm += 1
            for c in range(8):
                p.mm(ps_[:, 0:G], wb[:, c, m * 128:(m + 1) * 128], hb[:, c, :], start=(c == 0), stop=(c == 7))
            o = ofm[k_of % 3]
            if k_of % 2 == 0:
                p.copy(o[:, :], ps_[:, 0:G], eng="act")
            else:
                p.copy(o[:, :], ps_[:, 0:G], eng="dve")
            k_of += 1
            p.dma(zfm[m * 128:(m + 1) * 128, gi * G:(gi + 1) * G], o[:, :], is_out=True)
        for ti in range(TPG):
            t0 = gi * G + ti * 128
            off = CF
            for si, (nm, w, dt) in enumerate(tm_segs):
                o = otm[si][ti % 2]
                for c0 in range(0, w, 512):
                    cw = min(512, w - c0)
                    ps_ = pm[k_pm % 4]
                    k_pm += 1
                    for c in range(8):
                        p.mm(ps_[:, 0:cw], hb[:, c, ti * 128:(ti + 1) * 128],
                             wb[:, c, off + c0:off + c0 + cw], start=(c == 0), stop=(c == 7))
                    if k_of % 2 == 0:
                        p.copy(o[:, c0:c0 + cw], ps_[:, 0:cw], eng="act")
                    else:
                        p.copy(o[:, c0:c0 + cw], ps_[:, 0:cw], eng="dve")
                    k_of += 1
                p.dma(ztm[si][t0:t0 + 128, :], o[:, :], is_out=True)
                off += w
    return p


def build_outproj(NT, G=512):
    p = Prog()
    x = p.dram("x", [NT, D], F32, "ExternalInput")
    oT = p.dram("oT", [D, NT], BF16, "ExternalInput")
    Wo = p.dram("Wo", [D, D], F32, "ExternalInput")
    h1 = p.dram("h1", [NT, D], F32, "ExternalOutput")
    wb = p.sb("wb", [128, 8, D], BF16)
    stage = [p.sb("stage%d" % i, [128, 1024], F32) for i in range(2)]
    ot = [p.sb("ot%d" % i, [128, 8, G], BF16) for i in range(2)]
    xt = [p.sb("xt%d" % i, [128, D], F32) for i in range(3)]
    pm = [p.ps("pm%d" % i, [128, 512], F32) for i in range(4)]
    load_weight_bf16(p, Wo, wb, D, D, stage)
    k = 0
    it = 0
    for gi in range(NT // G):
        ob = ot[gi % 2]
        p.dma([ob[:, c, :] for c in range(8)],
              [oT[c * 128:(c + 1) * 128, gi * G:(gi + 1) * G] for c in range(8)])
        for ti in range(G // 128):
            t0 = gi * G + ti * 128
            xb = xt[it % 3]
            it += 1
            p.dma(xb[:, :], x[t0:t0 + 128, :])
            for half in range(2):
                ps_ = pm[k % 4]
                k += 1
                for c in range(8):
                    p.mm(ps_[:, :], ob[:, c, ti * 128:(ti + 1) * 128], wb[:, c, half * 512:(half + 1) * 512],
                         start=(c == 0), stop=(c == 7))
                p.tt(xb[:, half * 512:(half + 1) * 512], ps_[:, :], xb[:, half * 512:(half + 1) * 512], ALU.add)
            p.dma(h1[t0:t0 + 128, :], xb[:, :], is_out=True)
    return p


def build_ffn(NT, final=False, G=256):
    p = Prog()
    h1 = p.dram("h1", [NT, D], F32, "ExternalInput")
    Wg = p.dram("Wg", [D, FFN], F32, "ExternalInput")
    Wu = p.dram("Wu", [D, FFN], F32, "ExternalInput")
    Wd = p.dram("Wd", [FFN, D], F32, "ExternalInput")
    g_rep_d = p.dram("g_rep", [128, 1024], F32, "ExternalInput")
    ident_d = p.dram("ident", [128, 128], BF16, "ExternalInput")
    if final:
        gfin_d = p.dram("gfin", [128, D], F32, "ExternalInput")
    h2 = p.dram("h2", [NT, D], F32, "ExternalOutput")
    NF = FFN // 128
    wg = p.sb("wg", [128, 8, FFN], BF16)
    wu = p.sb("wu", [128, 8, FFN], BF16)
    wd = p.sb("wd", [128, NF, D], BF16)
    stage = [p.sb("stage%d" % i, [128, 1024], F32) for i in range(2)]
    g_rep = p.sb("g_rep_sb", [128, 1024], F32)
    ident = p.sb("ident_sb", [128, 128], BF16)
    TPG = G // 128
    xt = [p.sb("xt%d" % i, [128, D], F32) for i in range(TPG + 1)]
    scr = p.sb("scr", [128, D], BF16)
    xs = [p.sb("xs%d" % i, [128, D], BF16) for i in range(2)]
    ssq = [p.sb("ssq%d" % i, [128, 1], F32) for i in range(2)]
    rstd = [p.sb("rstd%d" % i, [128, 1], F32) for i in range(2)]
    hnT = [p.sb("hnT%d" % i, [128, 8, G], BF16) for i in range(2)]
    aT = p.sb("aT", [128, NF, G], BF16)
    sil = [p.sb("sil%d" % i, [128, G], F32) for i in range(2)]
    if final:
        gfin = p.sb("gfin_sb", [128, D], F32)
        ssq2 = p.sb("ssq2", [128, 1], F32)
        rstd2 = p.sb("rstd2", [128, 1], F32)
    pT = [p.ps("pT%d" % i, [128, 1024], BF16) for i in range(2)]
    pg = [p.ps("pg%d" % i, [128, 512], F32) for i in range(2)]
    pu = [p.ps("pu%d" % i, [128, 512], F32) for i in range(2)]
    pd = [p.ps("pd%d" % i, [128, 512], F32) for i in range(2)]

    p.dma(g_rep[:, :], g_rep_d[:, :])
    p.dma(ident[:, :], ident_d[:, :])
    if final:
        p.dma(gfin[:, :], gfin_d[:, :])
    load_weight_bf16(p, Wg, wg, D, FFN, stage)
    load_weight_bf16(p, Wu, wu, D, FFN, stage)
    load_weight_bf16(p, Wd, wd, FFN, D, stage)

    it = 0
    kf = 0
    kd = 0
    for gi in range(NT // G):
        hb = hnT[gi % 2]
        xts = []
        for ti in range(TPG):
            t0 = gi * G + ti * 128
            xb = xt[it % (TPG + 1)]
            b = it % 2
            it += 1
            xts.append(xb)
            p.dma(xb[:, :], h1[t0:t0 + 128, :])
            rms_to_fm(p, xb, hb, ti, g_rep, ident, scr, xs[b], pT[b], ssq[b], rstd[b])
        for f in range(NF):
            b = kf % 2
            kf += 1
            for c in range(8):
                p.mm(pg[b][:, 0:G], wg[:, c, f * 128:(f + 1) * 128], hb[:, c, :], start=(c == 0), stop=(c == 7))
            for c in range(8):
                p.mm(pu[b][:, 0:G], wu[:, c, f * 128:(f + 1) * 128], hb[:, c, :], start=(c == 0), stop=(c == 7))
            p.act(sil[b][:, :], pg[b][:, 0:G], AF.Silu)
            p.tt(aT[:, f, :], sil[b][:, :], pu[b][:, 0:G], ALU.mult)
        for ti in range(TPG):
            t0 = gi * G + ti * 128
            xb = xts[ti]
            for half in range(2):
                ps_ = pd[kd % 2]
                kd += 1
                for f in range(NF):
                    p.mm(ps_[:, :], aT[:, f, ti * 128:(ti + 1) * 128], wd[:, f, half * 512:(half + 1) * 512],
                         start=(f == 0), stop=(f == NF - 1))
                p.tt(xb[:, half * 512:(half + 1) * 512], ps_[:, :], xb[:, half * 512:(half + 1) * 512], ALU.add)
            if final:
                p.act(scr[:, :], xb[:, :], AF.Square, accum=ssq2[:, 0:1])
                p.act(rstd2[:, 0:1], ssq2[:, 0:1], AF.Sqrt, bias=EPS, scale=1.0 / D)
                p.recip(rstd2[:, 0:1], rstd2[:, 0:1])
                p.stt(xb[:, :], xb[:, :], rstd2[:, 0:1], gfin[:, :], ALU.mult, ALU.mult)
            p.dma(h2[t0:t0 + 128, :], xb[:, :], is_out=True)
    return p


NEG = -30000.0
NB = SEQ // 128
FM_QA, FM_KC, FM_VC, FM_KS, FM_KW, FM_QB, FM_KB = 0, 256, 320, 384, 448, 512, 768


def attn_consts():
    k = np.arange(128)[:, None]
    q = (np.arange(512) % 128)[None, :]
    c = {}
    c["ident"] = np.eye(128).astype(NPBF)
    c["mdiag"] = np.where(k <= q, 0.0, NEG).astype(NPBF)
    c["mfar"] = np.where(k > q, 0.0, NEG).astype(NPBF)
    cm = np.zeros((128, 17, 512), np.float32)
    for dl in range(17):
        cm[:, dl, :] = np.where(16 * k + 31 - q <= 128 * dl, 0.0, NEG)
    c["cmpmask"] = cm.reshape(128, 17 * 512).astype(NPBF)
    s = np.arange(128)[:, None]
    kk = np.arange(SEQ)[None, :]
    c["E"] = (kk // 64 == s).astype(np.float32).astype(NPBF)
    cs = np.arange(512)[:, None] * 16
    ss = np.arange(128)[None, :] * 64
    sm = ((cs < ss + 64) & (cs + 32 > ss)).astype(np.float32)
    sm[511, :] = 0.0
    c["selmap"] = sm.reshape(4, 128, 128).transpose(1, 0, 2).reshape(128, 512).astype(NPBF)
    ql = np.arange(128)[:, None]
    sp = np.arange(256)[None, :] - 126
    cur = ql // 64
    c["tb1"] = (sp < cur - 1).astype(np.float32)
    c["tb2"] = np.where(sp > cur, -1e30, np.where(sp >= cur - 1, 1e9, 0.0)).astype(np.float32)
    c["tb3"] = (sp <= cur).astype(np.float32)
    cv = np.ones((128, 4), np.float32)
    cv[127, 3] = 0.0
    c["cvalid"] = cv
    return c


def build_attn(nblk=NB):
    p = Prog()
    T_ = SEQ
    fm = p.dram("fm", [832, T_], BF16, "ExternalInput")
    tmv = p.dram("tmv", [T_, 192], BF16, "ExternalInput")
    gts = p.dram("gts", [T_, 12], F32, "ExternalInput")
    gbias_d = p.dram("gbias", [128, NB * 12], F32, "ExternalInput")
    sinks_d = p.dram("sinks", [128, 4], F32, "ExternalInput")
    posk_d = p.dram("posk", [64, 32], F32, "ExternalInput")
    posv_d = p.dram("posv", [64, 32], F32, "ExternalInput")
    w1k_d = p.dram("w1k", [64, 2048], F32, "ExternalInput")
    w1v_d = p.dram("w1v", [64, 2048], F32, "ExternalInput")
    w2k_d = p.dram("w2k", [64, 64], F32, "ExternalInput")
    w2v_d = p.dram("w2v", [64, 64], F32, "ExternalInput")
    cd = {}
    for nm, shp, dt in (("ident", [128, 128], BF16), ("mdiag", [128, 512], BF16), ("mfar", [128, 512], BF16),
                        ("cmpmask", [128, 17 * 512], BF16), ("E", [128, SEQ], BF16), ("selmap", [128, 512], BF16),
                        ("tb1", [128, 256], F32), ("tb2", [128, 256], F32), ("tb3", [128, 256], F32),
                        ("cvalid", [128, 4], F32)):
        d_ = p.dram(nm, shp, dt, "ExternalInput")
        s_ = p.sb(nm + "_sb", shp, dt)
        p.dma(s_[:, :], d_[:, :])
        cd[nm] = s_
    ident, mdiag, mfar, cmpmask, E, selmap = (cd[k] for k in ("ident", "mdiag", "mfar", "cmpmask", "E", "selmap"))
    tb1, tb2, tb3, cvalid = cd["tb1"], cd["tb2"], cd["tb3"], cd["cvalid"]
    oT = p.dram("oT", [512, T_], BF16, "ExternalOutput")

    ksT = p.sb("ksT", [64, T_], BF16)
    kwT = p.sb("kwT", [64, T_], BF16)
    kbT = p.sb("kbT", [64, T_], BF16)
    kcin = p.sb("kcin", [64, T_], BF16)
    vcin = p.sb("vcin", [64, T_], BF16)
    for dst, r0 in ((ksT, FM_KS), (kwT, FM_KW), (kbT, FM_KB), (kcin, FM_KC), (vcin, FM_VC)):
        p.dma([dst[:, i * 2048:(i + 1) * 2048] for i in range(4)],
              [fm[r0:r0 + 64, i * 2048:(i + 1) * 2048] for i in range(4)])
    vs1 = p.sb("vs1", [128, NB, 65], BF16)
    vw1 = p.sb("vw1", [128, NB, 65], BF16)
    vb1 = p.sb("vb1", [128, NB, 65], BF16)
    for i, vt in enumerate((vs1, vw1, vb1)):
        p.memset(vt[:, :, :], 1.0, eng="pool")
        src = tmv.h[:, i * 64:(i + 1) * 64].rearrange("(j p) d -> p j d", p=128)
        p.dma([vt[:, j * 8:(j + 1) * 8, 0:64] for j in range(8)],
              [tmv.v(src[:, j * 8:(j + 1) * 8, :]) for j in range(8)])
    gate = p.sb("gate", [128, NB * 12], F32)
    gb = p.sb("gb", [128, NB * 12], F32)
    p.dma(gate.v(gate.h[:, :].rearrange("p (j c) -> p j c", c=12)),
          gts.v(gts.h[:, :].rearrange("(j p) c -> p j c", p=128)))
    p.dma(gb[:, :], gbias_d[:, :])
    p.tt(gate[:, :], gate[:, :], gb[:, :], ALU.add)
    p.act(gate[:, :], gate[:, :], AF.Exp, scale=-1.0)
    p.ts(gate[:, :], gate[:, :], 1.0, None, ALU.add)
    p.recip(gate[:, :], gate[:, :])
    gate3 = gate.h[:, :].rearrange("p (j h c) -> p j h c", h=4, c=3)
    esink = p.sb("esink", [128, 4], F32)
    p.dma(esink[:, :], sinks_d[:, :])
    p.act(esink[:, :], esink[:, :], AF.Exp)

    S = [p.ps("S%d" % i, [128, 512], F32) for i in range(2)]
    Oc = p.ps("Oc", [128, 512], F32)
    U = p.ps("U", [128, 512], F32)
    Os = p.ps("Os", [128, 512], F32)
    Ow = p.ps("Ow", [128, 512], F32)
    Ob = p.ps("Ob", [128, 512], F32)
    pTr = p.ps("pTr", [128, 1024], BF16)

    kcT = p.sb("kcT", [64, 512], BF16)
    vc1 = p.sb("vc1", [128, 4, 65], BF16)
    stg = p.sb("stg", [64, 2048], F32)
    w1b = p.sb("w1b", [64, 2048], BF16)
    w2s = p.sb("w2s", [64, 64], F32)
    w2b = p.sb("w2b", [64, 64], BF16)
    poss = p.sb("poss", [64, 32], F32)
    posb = p.sb("posb", [64, 32], BF16)
    bcol = p.sb("bcol", [64, 1], F32)
    h1T = p.sb("h1T", [64, 512], BF16)
    p.memset(h1T[:, :], 0.0)
    p.memset(kcT[:, :], 0.0)
    p.memset(vc1[:, :, :], 0.0)
    for which, (w1d, w2d, posd, src) in enumerate(((w1k_d, w2k_d, posk_d, kcin), (w1v_d, w2v_d, posv_d, vcin))):
        p.dma(stg[:, :], w1d[:, :])
        p.copy(w1b[:, :], stg[:, :])
        p.dma(w2s[:, :], w2d[:, :])
        p.copy(w2b[:, :], w2s[:, :])
        p.dma(poss[:, :], posd[:, :])
        p.copy(posb[:, :], poss[:, :])
        for l in range(32):
            p.mm(S[0][0:64, 0:1], w1b[:, l * 64:(l + 1) * 64], posb[:, l:l + 1], start=(l == 0), stop=(l == 31))
        p.copy(bcol[:, :], S[0][0:64, 0:1])
        sv = src.h[:, :].rearrange("p (i r) -> p i r", r=16)
        for l in range(32):
            rhs = sv[:, 0:511, l] if l < 16 else sv[:, 1:512, l - 16]
            p.mm(S[1][0:64, 0:511], w1b[:, l * 64:(l + 1) * 64], src.v(rhs), start=(l == 0), stop=(l == 31))
        p.act(h1T[:, 0:511], S[1][0:64, 0:511], AF.Silu, bias=bcol[:, 0:1])
        if which == 0:
            p.mm(S[0][0:64, 0:511], w2b[:, :], h1T[:, 0:511])
            p.copy(kcT[:, 0:511], S[0][0:64, 0:511])
        else:
            for m in range(4):
                p.mm(S[0][:, m * 64:(m + 1) * 64], h1T[:, m * 128:(m + 1) * 128], w2b[:, :])
            p.copy(vc1[:, :, 0:64], S[0].v(S[0].h[:, 0:256].rearrange("p (m d) -> p m d", m=4)))
            p.copy(vc1[:, :, 64], cvalid[:, :])

    qa = [p.sb("qa%d" % i, [64, 512], BF16) for i in range(3)]
    qb = [p.sb("qb%d" % i, [64, 512], BF16) for i in range(2)]
    Pb = [p.sb("P%d" % i, [128, 512], BF16) for i in range(3)]
    nmT = [p.sb("nmT%d" % i, [128, 512], BF16) for i in range(2)]
    oacc = [p.sb("oacc%d" % i, [128, 512], F32) for i in range(2)]
    obf = p.sb("obf", [128, 512], BF16)
    oTs = [p.sb("oTs%d" % i, [128, 512], BF16) for i in range(2)]
    imp = p.sb("imp", [128, 128], F32)
    score = p.sb("score", [128, 128], F32)
    sc2 = p.sb("sc2", [128, 128], F32)
    m8 = p.sb("m8", [128, 8], F32)
    thr = p.sb("thr", [128, 1], F32)
    sel = p.sb("sel", [128, 128], F32)
    nmb = p.sb("nmb", [128, 128], BF16)
    lt = [p.sb("lt%d" % i, [128, 4], F32) for i in range(4)]
    wg_ = [p.sb("wgt%d" % i, [128, 4], F32) for i in range(4)]
    st = {"S": 0, "P": 0}

    def load_q(n):
        cs = slice(n * 128, (n + 1) * 128)
        a = qa[n % 3]
        p.dma(a.v(a.h[:, :].rearrange("d (h t) -> d h t", h=4)),
              fm.v(fm.h[FM_QA:FM_QA + 256, cs].rearrange("(h d) t -> d h t", d=64)))
        b = qb[n % 2]
        p.dma(b.v(b.h[:, :].rearrange("d (h t) -> d h t", h=4)),
              fm.v(fm.h[FM_QB:FM_QB + 256, cs].rearrange("(h d) t -> d h t", d=64)))

    def branch(specs, O, extra=None):
        nt = len(specs)
        banks = {}

        def emitS(i):
            ps_ = S[st["S"] % 2]
            st["S"] += 1
            banks[i] = ps_
            mms = specs[i][0]
            for idx, (l, r) in enumerate(mms):
                p.mm(ps_[:, :], l, r, start=(idx == 0), stop=(idx == len(mms) - 1))
        emitS(0)
        for i in range(nt):
            if i + 1 < nt:
                emitS(i + 1)
            P_ = Pb[st["P"] % 3]
            st["P"] += 1
            p.act(P_[:, :], banks[i][:, :], AF.Exp, scale=0.125)
            for h in range(4):
                p.mm(O[:, h * 65:(h + 1) * 65], P_[:, h * 128:(h + 1) * 128], specs[i][1],
                     start=(i == 0 and h == 0), stop=(i == nt - 1), skip=True)
            if extra is not None:
                extra(P_, i, i == 0, i == nt - 1)

    def ovw(O):
        return O.v(O.h[:, 0:260].rearrange("p (h e) -> p h e", e=65))

    def norm_weights(O, k, gate_j, n):
        if gate_j is None:
            p.tt(lt[k][:, :], ovw(O).tile.v(ovw(O).ap[:, :, 64]), esink[:, :], ALU.add)
        else:
            p.ts(lt[k][:, :], O.v(ovw(O).ap[:, :, 64]), 1e-30, None, ALU.max)
        p.recip(lt[k][:, :], lt[k][:, :])
        if gate_j is None:
            return lt[k]
        p.tt(wg_[k][:, :], lt[k][:, :], gate.v(gate3[:, n, :, gate_j]), ALU.mult)
        return wg_[k]

    def cmp_and_topk(n):
        a = qa[n % 3]
        ntc = min(4, n // 16 + 1)
        specs = []
        for m in range(ntc):
            mms = [(kcT[:, m * 128:(m + 1) * 128], a[:, :])]
            dl = n - 16 * m
            if dl <= 16:
                mms.append((ident[:, :], cmpmask[:, dl * 512:(dl + 1) * 512]))
            specs.append((mms, vc1[:, m, :]))

        def extra(P_, i, first, last):
            for h in range(4):
                p.mm(U[:, h * 128:(h + 1) * 128], P_[:, h * 128:(h + 1) * 128], selmap[:, i * 128:(i + 1) * 128],
                     start=(first and h == 0), stop=last, skip=True)
        branch(specs, Oc, extra)
        w = norm_weights(Oc, 0, 0, n)
        oa = oacc[n % 2]
        for h in range(4):
            p.act(oa[:, h * 64:(h + 1) * 64], Oc[:, h * 65:h * 65 + 64], AF.Copy, scale=w[:, h:h + 1])
        rl = lt[0]
        p.ts(imp[:, :], U[:, 0:128], rl[:, 0:1], None, ALU.mult)
        for h in range(1, 4):
            p.stt(imp[:, :], U[:, h * 128:(h + 1) * 128], rl[:, h:h + 1], imp[:, :], ALU.mult, ALU.add)
        u0 = 126 - 2 * n
        p.tt(score[:, :], imp[:, :], tb1[:, u0:u0 + 128], ALU.mult)
        p.tt(score[:, :], score[:, :], tb2[:, u0:u0 + 128], ALU.add)
        p.memset(score[:, 0:1], 1e9)
        so, s2o, m8o = score.h[:, :], sc2.h[:, :], m8.h[:, :]
        p.op("dve", lambda e: e.max(out=m8o, in_=so), [m8.all()], [score.all()])
        p.op("dve", lambda e: e.match_replace(out=s2o, in_to_replace=m8o, in_values=so, imm_value=-3e38),
             [sc2.all()], [m8.all(), score.all()])
        p.op("dve", lambda e: e.max(out=m8o, in_=s2o), [m8.all()], [sc2.all()])
        tho = thr.h[:, :]
        p.op("dve", lambda e: e.tensor_reduce(tho, m8o, AX.X, ALU.min), [thr.all()], [m8.all()])
        p.stt(sel[:, :], score[:, :], thr[:, 0:1], tb3[:, u0:u0 + 128], ALU.is_ge, ALU.mult)
        p.ts(nmb[:, :], sel[:, :], 1.0, -NEG, ALU.subtract, ALU.mult)
        p.tr(pTr[:, 0:128], nmb[:, :], ident[:, :])
        nt_ = nmT[n % 2]
        for h in range(4):
            p.copy(nt_[:, h * 128:(h + 1) * 128], pTr[:, 0:128], eng=("act" if h % 2 else "dve"))

    def rest(n):
        a = qa[n % 3]
        b = qb[n % 2]
        nt_ = nmT[n % 2]
        oa = oacc[n % 2]
        ks_ = lambda j: slice(j * 128, (j + 1) * 128)
        specs = []
        for j in range(n + 1):
            mms = [(ksT[:, ks_(j)], a[:, :]), (E[:, ks_(j)], nt_[:, :])]
            if j == n:
                mms.append((ident[:, :], mdiag[:, :]))
            specs.append((mms, vs1[:, j, :]))
        branch(specs, Os)
        specs = []
        for j in range(max(0, n - 4), n + 1):
            mms = [(kwT[:, ks_(j)], a[:, :])]
            if j == n:
                mms.append((ident[:, :], mdiag[:, :]))
            if j == n - 4:
                mms.append((ident[:, :], mfar[:, :]))
            specs.append((mms, vw1[:, j, :]))
        branch(specs, Ow)
        specs = []
        for j in range(max(0, n - 1), n + 1):
            mms = [(kbT[:, ks_(j)], b[:, :])]
            if j == n:
                mms.append((ident[:, :], mdiag[:, :]))
            if j == n - 1:
                mms.append((ident[:, :], mfar[:, :]))
            specs.append((mms, vb1[:, j, :]))
        branch(specs, Ob)
        w1_ = norm_weights(Os, 1, 1, n)
        for h in range(4):
            p.stt(oa[:, h * 64:(h + 1) * 64], Os[:, h * 65:h * 65 + 64], w1_[:, h:h + 1],
                  oa[:, h * 64:(h + 1) * 64], ALU.mult, ALU.add)
        w2_ = norm_weights(Ow, 2, 2, n)
        for h in range(4):
            p.stt(obf[:, h * 64:(h + 1) * 64], Ow[:, h * 65:h * 65 + 64], w2_[:, h:h + 1],
                  oa[:, h * 64:(h + 1) * 64], ALU.mult, ALU.add)
        w3_ = norm_weights(Ob, 3, None, n)
        for h in range(4):
            p.act(obf[:, 256 + h * 64:256 + (h + 1) * 64], Ob[:, h * 65:h * 65 + 64], AF.Copy, scale=w3_[:, h:h + 1])
        for c in range(4):
            p.tr(pTr[:, 512 + c * 128:512 + (c + 1) * 128], obf[:, c * 128:(c + 1) * 128], ident[:, :])
        ot = oTs[n % 2]
        p.copy(ot[:, :], pTr[:, 512:1024], eng="dve")
        p.dma([oT[c * 128:(c + 1) * 128, n * 128:(n + 1) * 128] for c in range(4)],
              [ot[:, c * 128:(c + 1) * 128] for c in range(4)], is_out=True)

    load_q(0)
    cmp_and_topk(0)
    for n in range(nblk):
        if n + 1 < nblk:
            load_q(n + 1)
            cmp_and_topk(n + 1)
        rest(n)
    return p


def ab_perm():
    off = dict(qa=0, kc=512, vc=640, ks=768, vs=896, kw=1024, vw=1152, g=1280, qb=1304, kb=1816, vb=1944)
    fmc = []
    for g in range(2):
        fmc += list(range(off["qa"] + g * 256, off["qa"] + (g + 1) * 256))
        for nm in ("kc", "vc", "ks", "kw"):
            fmc += list(range(off[nm] + g * 64, off[nm] + (g + 1) * 64))
        fmc += list(range(off["qb"] + g * 256, off["qb"] + (g + 1) * 256))
        fmc += list(range(off["kb"] + g * 64, off["kb"] + (g + 1) * 64))
    tmc = []
    for g in range(2):
        for nm in ("vs", "vw", "vb"):
            tmc += list(range(off[nm] + g * 64, off[nm] + (g + 1) * 64))
    gc = list(range(1280, 1304))
    return fmc, tmc, gc


def g_rep_of(g):
    return np.ascontiguousarray(
        np.repeat(np.asarray(g, np.float32).reshape(8, 128).T[:, :, None], 128, axis=2).reshape(128, 1024))


def attn_inputs(g, fm, tmv, gts, gate_bias, sinks, pos_k, pos_v, w1k, w2k, w1v, w2v, consts):
    d = dict(consts)
    d["fm"] = np.ascontiguousarray(fm)
    d["tmv"] = np.ascontiguousarray(tmv)
    d["gts"] = np.ascontiguousarray(gts)
    d["gbias"] = np.ascontiguousarray(np.tile(np.asarray(gate_bias[g * 12:(g + 1) * 12], np.float32)[None, :], (128, NB)))
    d["sinks"] = np.ascontiguousarray(np.broadcast_to(np.asarray(sinks[g * 4:(g + 1) * 4], np.float32)[None, :], (128, 4)))
    d["posk"] = np.ascontiguousarray(np.asarray(pos_k, np.float32).T)
    d["posv"] = np.ascontiguousarray(np.asarray(pos_v, np.float32).T)
    r1 = lambda w: np.ascontiguousarray(np.asarray(w, np.float32).reshape(32, 64, 64).transpose(1, 0, 2).reshape(64, 2048))
    d["w1k"] = r1(w1k)
    d["w1v"] = r1(w1v)
    d["w2k"] = np.ascontiguousarray(np.asarray(w2k, np.float32))
    d["w2v"] = np.ascontiguousarray(np.asarray(w2v, np.float32))
    return d


def mlstm_consts():
    s = np.arange(128)[:, None]
    t = np.arange(128)[None, :]
    c = {}
    c["ident"] = np.eye(128).astype(NPBF)
    c["tri"] = (s <= t).astype(np.float32)
    c["ones"] = np.ones((128, 128), np.float32)
    return c


def build_mlstm(nch=SEQ // 128, dbg=99):
    p = Prog()
    T_ = SEQ
    NCH = SEQ // 128
    qkfm = p.dram("qkfm", [512, T_], F32, "ExternalInput")
    vtm = p.dram("vtm", [T_, 512], BF16, "ExternalInput")
    ogtm = p.dram("ogtm", [T_, 512], F32, "ExternalInput")
    gtm = p.dram("gtm", [T_, 4], F32, "ExternalInput")
    cw_d = p.dram("cw", [128, 16], F32, "ExternalInput")
    cb_d = p.dram("cb", [128, 4], F32, "ExternalInput")
    ib_d = p.dram("ibf", [128, NCH * 2], F32, "ExternalInput")
    fb_d = p.dram("fbf", [128, NCH * 2], F32, "ExternalInput")
    hhT = p.dram("hhT", [512, T_], BF16, "ExternalOutput")
    cd = {}
    for nm, shp, dt in (("ident", [128, 128], BF16), ("tri", [128, 128], F32), ("ones", [128, 128], F32),
                        ("cw", None, None), ("cb", None, None), ("ibf", None, None), ("fbf", None, None)):
        if shp is None:
            d_ = {"cw": cw_d, "cb": cb_d, "ibf": ib_d, "fbf": fb_d}[nm]
            shp, dt = list(d_.h.shape), F32
        else:
            d_ = p.dram(nm, shp, dt, "ExternalInput")
        s_ = p.sb(nm + "_sb", shp, dt)
        p.dma(s_[:, :], d_[:, :])
        cd[nm] = s_
    ident, tri, ones, cw, cb, ibf, fbf = (cd[k] for k in ("ident", "tri", "ones", "cw", "cb", "ibf", "fbf"))

    A = [p.ps("A%d" % i, [128, 512], F32) for i in range(2)]
    B = [p.ps("B%d" % i, [128, 512], F32) for i in range(2)]
    Cn = [p.ps("Cn%d" % i, [128, 512], F32) for i in range(2)]
    pTk = p.ps("pTk", [128, 1024], BF16)
    pTh = p.ps("pTh", [128, 1024], BF16)

    gt = p.sb("gt", [128, NCH * 4], F32)
    p.dma(gt.v(gt.h[:, :].rearrange("p (j c) -> p j c", c=4)),
          gtm.v(gtm.h[:, :].rearrange("(j p) c -> p j c", p=128)))
    gt3 = gt.h[:, :].rearrange("p (j c) -> p j c", c=4)
    icb = p.sb("icb", [128, NCH * 2], F32)
    sp = p.sb("sp", [128, NCH * 2], F32)
    v3 = lambda t: t.h[:, :].rearrange("p (j c) -> p j c", c=2)
    p.tt(icb.v(v3(icb)), gt.v(gt3[:, :, 0:2]), ibf.v(v3(ibf)), ALU.add)
    p.tt(sp.v(v3(sp)), gt.v(gt3[:, :, 2:4]), fbf.v(v3(fbf)), ALU.add)
    if dbg == -1:
        return p
    p.act(sp[:, :], sp[:, :], AF.Exp, scale=-1.0)
    p.act(sp[:, :], sp[:, :], AF.Ln, bias=1.0)
    NC2 = NCH * 2
    if dbg == -2:
        return p
    p.mm(A[0][:, 0:NC2], tri[:, :], sp[:, :])
    p.mm(A[1][:, 0:NC2], ones[:, :], sp[:, :])
    eb = p.sb("eb", [128, NC2], F32)
    eu = p.sb("eu", [128, NC2], F32)
    ebl = p.sb("ebl", [128, NC2], F32)
    if dbg == -3:
        return p
    p.act(eb[:, :], A[0][:, 0:NC2], AF.Exp, scale=-1.0)
    if dbg == -4:
        return p
    p.tt(eu[:, :], icb[:, :], A[0][:, 0:NC2], ALU.add)
    if dbg == -5:
        return p
    p.act(eu[:, :], eu[:, :], AF.Exp)
    if dbg == -6:
        return p
    p.act(ebl[:, :], A[1][:, 0:NC2], AF.Exp, scale=-1.0)
    if dbg == -7:
        return p
    eub = p.sb("eub", [128, NC2], BF16)
    p.copy(eub[:, :], eu[:, :])

    if dbg == 0:
        return p
    qk = p.sb("qk", [128, 4, T_], BF16)
    xin = p.sb("xin", [128, T_ + 3], F32)
    acc = p.sb("acc", [128, T_], F32)
    p.memset(xin[:, 0:3], 0.0)
    HT = T_ // 2
    for c in range(min(4, dbg)):
        p.dma([xin[:, 3 + i * 2048:3 + (i + 1) * 2048] for i in range(4)],
              [qkfm[c * 128:(c + 1) * 128, i * 2048:(i + 1) * 2048] for i in range(4)])
        for hf in range(2):
            o0 = hf * HT
            e_ = "dve"
            p.ts(acc[:, o0:o0 + HT], xin[:, 3 + o0:3 + o0 + HT], cw[:, c * 4 + 3:c * 4 + 4], None, ALU.mult, eng=e_)
            for j in range(3):
                p.stt(acc[:, o0:o0 + HT], xin[:, j + o0:j + o0 + HT], cw[:, c * 4 + j:c * 4 + j + 1],
                      acc[:, o0:o0 + HT], ALU.mult, ALU.add, eng=e_)
        if c < 2:
            p.act(qk[:, c, :], acc[:, :], AF.Silu, bias=cb[:, c:c + 1])
        else:
            p.act(acc[:, :], acc[:, :], AF.Silu, bias=cb[:, c:c + 1])
            p.ts(qk[:, c, :], acc[:, :], 128.0 ** -0.5, None, ALU.mult)

    CN = [p.sb("CN%d" % i, [128, 257], F32) for i in range(2)]
    CNs = [p.sb("CNs%d" % i, [128, 257], F32) for i in range(2)]
    CNb = [p.sb("CNb%d" % i, [128, 257], BF16) for i in range(2)]
    for i in range(2):
        p.memset(CN[i][:, :], 0.0)
        p.memset(CNb[i][:, :], 0.0)
    vraw = [p.sb("vraw%d" % i, [128, 512], BF16) for i in range(3)]
    ogt = [p.sb("ogt%d" % i, [128, 512], F32) for i in range(3)]
    vp = [p.sb("vp%d" % i, [128, 257], BF16) for i in range(4)]
    PT = [p.sb("PT%d" % i, [128, 128], BF16) for i in range(4)]
    ktm = [p.sb("ktm%d" % i, [128, 128], BF16) for i in range(4)]
    dn = [p.sb("dn%d" % i, [128, 1], F32) for i in range(4)]
    dn2 = [p.sb("dnb%d" % i, [128, 1], F32) for i in range(4)]
    hbf = [p.sb("hbf%d" % i, [128, 256], BF16) for i in range(4)]
    hTs = [p.sb("hTs%d" % i, [128, 256], BF16) for i in range(4)]
    k = 0
    for j in range(nch):
        cs = slice(j * 128, (j + 1) * 128)
        vr = vraw[j % 3]
        og = ogt[j % 3]
        p.dma(vr[:, :], vtm[cs, :])
        p.dma(og[:, :], ogtm[cs, :])
        p.act(og[:, :], og[:, :], AF.Exp, scale=-1.0)
        p.ts(og[:, :], og[:, :], 1.0, None, ALU.add, eng="pool")
        p.recip(og[:, :], og[:, :])
        for hd in range(2):
            col = j * 2 + hd
            b4 = k % 4
            k += 1
            qT = qk[:, hd, cs]
            kT = qk[:, 2 + hd, cs]
            p.mm(A[hd][:, 0:128], kT, qT)
            p.tt(PT[b4][:, :], A[hd][:, 0:128], tri[:, :], ALU.mult)
            p.tr(pTk[:, hd * 128:(hd + 1) * 128], kT, ident[:, :])
            p.copy(ktm[b4][:, :], pTk[:, hd * 128:(hd + 1) * 128], eng="act")
            p.act(vp[b4][:, 0:256], vr[:, hd * 256:(hd + 1) * 256], AF.Copy, scale=eu[:, col:col + 1])
            p.copy(vp[b4][:, 256:257], eub[:, col:col + 1], eng="pool")
            p.mm(B[hd][:, 0:257], qT, CNb[hd][:, :], start=True, stop=False)
            p.mm(B[hd][:, 0:257], PT[b4][:, :], vp[b4][:, :], start=False, stop=True)
            p.mm(Cn[hd][:, 0:257], ktm[b4][:, :], vp[b4][:, :])
            p.tt(CNs[hd][:, :], Cn[hd][:, 0:257], CN[hd][:, :], ALU.add)
            p.ts(CN[hd][:, :], CNs[hd][:, :], ebl[:, col:col + 1], None, ALU.mult, eng="pool")
            p.act(CNb[hd][:, :], CNs[hd][:, :], AF.Copy, scale=ebl[:, col:col + 1])
            d_ = dn[b4]
            p.tt(d_[:, :], B[hd][:, 256:257], eb[:, col:col + 1], ALU.mult)
            p.stt(dn2[b4][:, :], d_[:, :], -1.0, d_[:, :], ALU.mult, ALU.max)
            p.ts(d_[:, :], dn2[b4][:, :], 1.0, None, ALU.max)
            p.recip(d_[:, :], d_[:, :])
            p.tt(d_[:, :], d_[:, :], eb[:, col:col + 1], ALU.mult)
            p.stt(hbf[b4][:, :], B[hd][:, 0:256], d_[:, 0:1], og[:, hd * 256:(hd + 1) * 256], ALU.mult, ALU.mult)
            for c in range(2):
                p.tr(pTh[:, hd * 256 + c * 128:hd * 256 + (c + 1) * 128], hbf[b4][:, c * 128:(c + 1) * 128], ident[:, :])
            p.copy(hTs[b4][:, :], pTh[:, hd * 256:(hd + 1) * 256], eng="act")
            p.dma([hhT[hd * 256 + c * 128:hd * 256 + (c + 1) * 128, cs] for c in range(2)],
                  [hTs[b4][:, c * 128:(c + 1) * 128] for c in range(2)], is_out=True)
    return p


def c_perm():
    fmc = []
    for hp in range(2):
        for base in (0, 512):
            for hd in range(2):
                h = 2 * hp + hd
                fmc += list(range(base + h * 128, base + (h + 1) * 128))
    vcols = list(range(1024, 2048))
    ogcols = list(range(2048, 3072))
    gcols = list(range(3072, 3080))
    return fmc, vcols, ogcols, gcols


def mlstm_inputs(hp, qkfm, vtm, ogtm, gtm, conv_w, conv_b, ib, fb, consts):
    d = dict(consts)
    d["qkfm"] = np.ascontiguousarray(qkfm)
    d["vtm"] = np.ascontiguousarray(vtm)
    d["ogtm"] = np.ascontiguousarray(ogtm)
    d["gtm"] = np.ascontiguousarray(gtm)
    fmc, _, _, _ = c_perm()
    ch = np.asarray(fmc[hp * 512:(hp + 1) * 512]).reshape(4, 128)
    cwv = np.asarray(conv_w, np.float32)[:, ch]
    d["cw"] = np.ascontiguousarray(cwv.transpose(2, 1, 0).reshape(128, 16))
    d["cb"] = np.ascontiguousarray(np.asarray(conv_b, np.float32)[ch].T)
    nchk = SEQ // 128
    d["ibf"] = np.ascontiguousarray(np.tile(np.asarray(ib[2 * hp:2 * hp + 2], np.float32)[None, :], (128, nchk)))
    d["fbf"] = np.ascontiguousarray(np.tile(np.asarray(fb[2 * hp:2 * hp + 2], np.float32)[None, :], (128, nchk)))
    return d


def _run(p, in_maps):
    return run_prog(p, in_maps)


def kernel(x, norm_mix, norm_ffn, ffn_w_gate, ffn_w_up, ffn_w_down, ab_w_in, ab_gate_bias, nsa_pos_k, nsa_pos_v,
           nsa_cmp_k_w1, nsa_cmp_k_w2, nsa_cmp_v_w1, nsa_cmp_v_w2, swa_sinks, ab_w_out, c_w_in, c_conv_w, c_conv_b,
           c_igate_bias, c_fgate_bias, c_w_out, final_norm):
    f32 = lambda a: np.ascontiguousarray(np.asarray(a, np.float32))
    x = f32(x)
    NT = BATCH * SEQ // NCORES
    xr = x.reshape(BATCH * SEQ, D)
    ident = np.eye(128).astype(NPBF)
    rows = lambda a, c: np.ascontiguousarray(a[c * NT:(c + 1) * NT])

    fmc, tmc, gc = ab_perm()
    W0 = f32(np.asarray(ab_w_in[0])[:, fmc + tmc + gc])
    g0 = g_rep_of(norm_mix[0])
    p = build_proj(NT, 1664, BF16, [("v", 384, BF16), ("g", 24, F32)])
    r = _run(p, [dict(h=rows(xr, c), W=W0, g_rep=g0, ident=ident) for c in range(NCORES)])
    consts = attn_consts()
    ins = []
    for c in range(NCORES):
        b, g = c // 2, c % 2
        fm = np.concatenate([np.asarray(r[2 * b + hf]["zfm"])[g * 832:(g + 1) * 832] for hf in range(2)], axis=1)
        tmv = np.concatenate([np.asarray(r[2 * b + hf]["ztm_v"])[:, g * 192:(g + 1) * 192] for hf in range(2)], axis=0)
        gts = np.concatenate([np.asarray(r[2 * b + hf]["ztm_g"])[:, g * 12:(g + 1) * 12] for hf in range(2)], axis=0)
        ins.append(attn_inputs(g, fm, tmv, gts, np.asarray(ab_gate_bias[0]), np.asarray(swa_sinks[0]),
                               nsa_pos_k[0], nsa_pos_v[0], nsa_cmp_k_w1[0], nsa_cmp_k_w2[0],
                               nsa_cmp_v_w1[0], nsa_cmp_v_w2[0], consts))
    p = build_attn()
    r = _run(p, ins)
    ins = []
    for c in range(NCORES):
        b, hf = c // 2, c % 2
        cs = slice(hf * NT, (hf + 1) * NT)
        o0 = np.asarray(r[2 * b]["oT"])
        o1 = np.asarray(r[2 * b + 1]["oT"])
        oT = np.concatenate([o0[0:256, cs], o1[0:256, cs], o0[256:512, cs], o1[256:512, cs]], axis=0)
        ins.append(dict(x=rows(xr, c), oT=np.ascontiguousarray(oT), Wo=f32(ab_w_out[0])))
    p = build_outproj(NT)
    r = _run(p, ins)
    p = build_ffn(NT, final=False)
    gf0 = g_rep_of(norm_ffn[0])
    r = _run(p, [dict(h1=np.asarray(r[c]["h1"]), Wg=f32(ffn_w_gate[0]), Wu=f32(ffn_w_up[0]), Wd=f32(ffn_w_down[0]),
                      g_rep=gf0, ident=ident) for c in range(NCORES)])
    h2 = [np.asarray(r[c]["h2"]) for c in range(NCORES)]
    fmc1, vcols, ogcols, gcols = c_perm()
    W1 = f32(np.asarray(c_w_in[0])[:, fmc1 + vcols + ogcols + gcols])
    p = build_proj(NT, 1024, F32, [("v", 1024, BF16), ("og", 1024, F32), ("g", 8, F32)])
    g1 = g_rep_of(norm_mix[1])
    r = _run(p, [dict(h=h2[c], W=W1, g_rep=g1, ident=ident) for c in range(NCORES)])
    mc = mlstm_consts()
    ins = []
    for c in range(NCORES):
        b, hp = c // 2, c % 2
        qkfm = np.concatenate([np.asarray(r[2 * b + hf]["zfm"])[hp * 512:(hp + 1) * 512] for hf in range(2)], axis=1)
        vtm = np.concatenate([np.asarray(r[2 * b + hf]["ztm_v"])[:, hp * 512:(hp + 1) * 512] for hf in range(2)], axis=0)
        ogtm = np.concatenate([np.asarray(r[2 * b + hf]["ztm_og"])[:, hp * 512:(hp + 1) * 512] for hf in range(2)], axis=0)
        gsel = [2 * hp, 2 * hp + 1, 4 + 2 * hp, 4 + 2 * hp + 1]
        gtm = np.concatenate([np.asarray(r[2 * b + hf]["ztm_g"])[:, gsel] for hf in range(2)], axis=0)
        ins.append(mlstm_inputs(hp, qkfm, vtm, ogtm, gtm, c_conv_w[0], c_conv_b[0],
                                np.asarray(c_igate_bias[0]), np.asarray(c_fgate_bias[0]), mc))
    p = build_mlstm()
    r = _run(p, ins)
    ins = []
    for c in range(NCORES):
        b, hf = c // 2, c % 2
        cs = slice(hf * NT, (hf + 1) * NT)
        oT = np.concatenate([np.asarray(r[2 * b]["hhT"])[:, cs], np.asarray(r[2 * b + 1]["hhT"])[:, cs]], axis=0)
        ins.append(dict(x=h2[c], oT=np.ascontiguousarray(oT), Wo=f32(c_w_out[0])))
    p = build_outproj(NT)
    r = _run(p, ins)
    p = build_ffn(NT, final=True)
    gf1 = g_rep_of(norm_ffn[1])
    gfin = np.ascontiguousarray(np.broadcast_to(np.asarray(final_norm, np.float32)[None, :], (128, D)))
    r = _run(p, [dict(h1=np.asarray(r[c]["h1"]), Wg=f32(ffn_w_gate[1]), Wu=f32(ffn_w_up[1]), Wd=f32(ffn_w_down[1]),
                      g_rep=gf1, ident=ident, gfin=gfin) for c in range(NCORES)])
    out = np.concatenate([np.asarray(r[c]["h2"]) for c in range(NCORES)], axis=0)
    return out.reshape(BATCH, SEQ, D).astype(np.float32)
```

```python
import numpy as np
import ml_dtypes
from contextlib import ExitStack
import concourse.bass as bass
import concourse.mybir as mybir
from concourse.bass_utils import run_bass_kernel_spmd

F32 = mybir.dt.float32
BF16 = mybir.dt.bfloat16
AF = mybir.ActivationFunctionType
ALU = mybir.AluOpType
AX = mybir.AxisListType
NPBF = ml_dtypes.bfloat16

D = 1024
SEQ = 8192
BATCH = 4
NCORES = 8
FFN = 2816
EPS = 1e-6


class V:
    __slots__ = ("tile", "ap")

    def __init__(self, tile, ap):
        self.tile = tile
        self.ap = ap


class T:
    def __init__(self, prog, handle, name):
        self.p = prog
        self.h = handle
        self.name = name
        self.lw = None
        self.rd = []
        self.dsem = None
        self.dcnt = 0
        self.skey = None
        self.track = True
        self.onchip = True
        self.psum = False

    def __getitem__(self, idx):
        return V(self, self.h[idx])

    def v(self, ap):
        return V(self, ap)

    def all(self):
        return V(self, self.h[:])


ENG = ("pe", "act", "dve", "pool", "sp")


class Prog:
    def __init__(self):
        self.nc = bass.Bass("TRN2", target_bir_lowering=False)
        self.es = ExitStack()
        self.ops = {e: [] for e in ENG}
        self.cnt = {e: 0 for e in ENG}
        self.waited = {e: {} for e in ENG}
        self.sems = {}
        self.nsem = 0
        for e in ENG:
            self.sems[e] = self.es.enter_context(self.nc.semaphore("s_" + e))
        self.out_tokens = []
        self.uid = 0
        self.pes = None
        self.phase_tiles = []
        self.sempool = []
        self.semcount = {}
        self.barrier = {}
        self.dyn = {}
        self.emit_id = 0

    def dram(self, name, shape, dt, kind):
        if kind == "Internal":
            h = self.nc.dram_tensor(name, list(shape), dt)
        else:
            h = self.nc.dram_tensor(name, list(shape), dt, kind=kind)
        t = T(self, h, name)
        t.onchip = False
        t.track = (kind == "Internal")
        return t

    def _stack(self):
        return self.pes if self.pes is not None else self.es

    def sb(self, name, shape, dt):
        self.uid += 1
        h = self._stack().enter_context(self.nc.sbuf_tensor("%s_%d" % (name, self.uid), list(shape), dt))
        t = T(self, h, name)
        self.phase_tiles.append(t)
        return t

    def ps(self, name, shape, dt):
        self.uid += 1
        h = self._stack().enter_context(self.nc.psum_tensor("%s_%d" % (name, self.uid), list(shape), dt))
        t = T(self, h, name)
        t.psum = True
        self.phase_tiles.append(t)
        return t

    def _dsem(self, t):
        if t.dsem is None:
            if self.sempool:
                t.skey, t.dsem, t.dcnt = self.sempool.pop()
            else:
                self.nsem += 1
                t.skey = "d%d" % self.nsem
                t.dsem = self.es.enter_context(self.nc.semaphore(t.skey))
                t.dcnt = 0
                self.sems[t.skey] = t.dsem
        return t.dsem

    def begin_phase(self):
        self.pes = ExitStack()
        self.phase_tiles = []

    def end_phase(self):
        self.emit()
        for t in self.phase_tiles:
            if t.dsem is not None:
                self.sempool.append((t.skey, t.dsem, t.dcnt))
                t.dsem = None
        self.phase_tiles = []
        self.pes.close()
        self.pes = None
        bar = dict(self.semcount)
        for e in ENG:
            bar[e] = self.cnt[e]
        self.barrier = bar

    def op(self, eng, fn, writes, reads, dma=False, is_out=False, semtile=None, incamt=16):
        deps = {}

        def add(tok):
            if tok is None:
                return
            k, v = tok
            if deps.get(k, 0) < v:
                deps[k] = v

        wt = []
        for w in writes:
            if w.tile not in wt:
                wt.append(w.tile)
        rt = []
        for r in reads:
            if r.tile not in rt and r.tile not in wt:
                rt.append(r.tile)
        for t in rt:
            if t.track:
                add(t.lw)
                if t.psum:
                    for r in t.rd:
                        if r[0] != eng:
                            add(r)
        for t in wt:
            if t.track:
                add(t.lw)
                for r in t.rd:
                    add(r)
        waits = []
        for k, v in deps.items():
            if k == eng and eng == "pe" and not dma:
                continue
            if self.waited[eng].get(k, 0) >= v:
                continue
            self.waited[eng][k] = v
            waits.append((self.sems[k], v))
        if dma:
            n = dma if isinstance(dma, int) and dma is not True else 1
            sem = self._dsem(semtile)
            semtile.dcnt += incamt * n
            tok = (semtile.skey, semtile.dcnt)
            self.semcount[semtile.skey] = semtile.dcnt
            inc = (sem, incamt)
        else:
            self.cnt[eng] += 1
            tok = (eng, self.cnt[eng])
            inc = (self.sems[eng], 1)
        self.ops[eng].append((waits, fn, inc))
        for t in wt:
            if t.track:
                t.lw = tok
                t.rd = []
        for t in rt:
            if t.track:
                t.rd.append(tok)
        if is_out:
            self.out_tokens.append(tok)
        return tok

    def dma(self, out, in_, eng="sp", is_out=False):
        outs = out if isinstance(out, list) else [out]
        ins = in_ if isinstance(in_, list) else [in_]
        pairs = [(o.ap, i.ap) for o, i in zip(outs, ins)]
        semtile = outs[0].tile if outs[0].tile.onchip else ins[0].tile
        n = len(pairs)

        def fn(e):
            r = []
            for o, i in pairs:
                if callable(o):
                    o = o(e)
                if callable(i):
                    i = i(e)
                r.append(e.dma_start(out=o, in_=i))
            return r
        return self.op(eng, fn, outs, ins, dma=n, is_out=is_out, semtile=semtile)

    def collective(self, kind, out, in_, groups):
        o, i = out.ap, in_.ap

        def fn(e):
            return [e.collective_compute(kind, ALU.bypass, replica_groups=groups, ins=[i], outs=[o])]
        return self.op("pool", fn, [out], [in_], dma=1, semtile=out.tile, incamt=1)

    def mm(self, out, lhsT, rhs, start=True, stop=True, skip=False):
        o, l, r = out.ap, lhsT.ap, rhs.ap
        if skip:
            return self.op("pe", lambda e: e.matmul(o, l, r, start=start, stop=stop, skip_group_check=True),
                           [out], [lhsT, rhs])
        return self.op("pe", lambda e: e.matmul(o, l, r, start=start, stop=stop), [out], [lhsT, rhs])

    def tr(self, out, in_, ident):
        o, i, d = out.ap, in_.ap, ident.ap
        return self.op("pe", lambda e: e.transpose(o, i, d), [out], [in_, ident])

    def act(self, out, in_, func, bias=None, scale=None, accum=None, eng="act"):
        o, i = out.ap, in_.ap
        kw = {}
        rd = [in_]
        wr = [out]
        if bias is not None:
            if isinstance(bias, V):
                kw["bias"] = bias.ap
                rd.append(bias)
            else:
                kw["bias"] = bias
        if scale is not None:
            if isinstance(scale, V):
                kw["scale"] = scale.ap
                rd.append(scale)
            else:
                kw["scale"] = scale
        if accum is not None:
            kw["accum_out"] = accum.ap
            wr.append(accum)
        return self.op("act", lambda e: e.activation(o, i, func, **kw), wr, rd)

    def tt(self, out, a, b, op, eng="dve"):
        o, x, y = out.ap, a.ap, b.ap
        return self.op(eng, lambda e: e.tensor_tensor(o, x, y, op), [out], [a, b])

    def ts(self, out, a, s1, s2, op0, op1=None, eng="dve", accum=None):
        o, x = out.ap, a.ap
        rd = [a]
        wr = [out]
        if isinstance(s1, V):
            rd.append(s1)
            s1 = s1.ap
        if isinstance(s2, V):
            rd.append(s2)
            s2 = s2.ap
        kw = {}
        if op1 is not None:
            kw["op1"] = op1
        if accum is not None:
            kw["accum_out"] = accum.ap
            wr.append(accum)
        return self.op(eng, lambda e: e.tensor_scalar(o, x, s1, s2, op0, **kw), wr, rd)

    def stt(self, out, a, s, b, op0, op1, eng="dve"):
        o, x, y = out.ap, a.ap, b.ap
        rd = [a, b]
        if isinstance(s, V):
            rd.append(s)
            s = s.ap
        return self.op(eng, lambda e: e.scalar_tensor_tensor(o, x, s, y, op0, op1), [out], rd)

    def copy(self, out, in_, eng="dve"):
        o, i = out.ap, in_.ap
        if eng == "act":
            return self.op("act", lambda e: e.copy(o, i), [out], [in_])
        return self.op(eng, lambda e: e.tensor_copy(o, i), [out], [in_])

    def memset(self, out, val, eng="dve"):
        o = out.ap
        return self.op(eng, lambda e: e.memset(o, val), [out], [])

    def recip(self, out, in_):
        o, i = out.ap, in_.ap
        return self.op("dve", lambda e: e.reciprocal(o, i), [out], [in_])

    def emit(self, final=False):
        nc = self.nc
        self.emit_id += 1
        ops = self.ops
        sems = self.sems
        bar = self.barrier
        self.barrier = {}
        fin = {}
        if final:
            for k, v in self.out_tokens:
                fin[k] = max(fin.get(k, 0), v)
        engobj = {"pe": "tensor", "act": "scalar", "dve": "vector", "pool": "gpsimd", "sp": "sync"}
        waited = self.waited
        with nc.Block() as block:
            def mk(ename):
                def body(e):
                    for k, v in bar.items():
                        if v > 0 and k != ename:
                            e.wait_ge(sems[k], v)
                    for waits, fn, inc in ops[ename]:
                        for s_, v in waits:
                            e.wait_ge(s_, v)
                        r = fn(e)
                        if isinstance(r, list):
                            for x in r:
                                x.then_inc(inc[0], inc[1])
                        else:
                            r.then_inc(inc[0], inc[1])
                    if ename == "sp":
                        for k, v in fin.items():
                            e.wait_ge(sems[k], v)
                return body
            for ename in ENG:
                getattr(block, engobj[ename])(mk(ename))
        for ename in ENG:
            for k, v in bar.items():
                if waited[ename].get(k, 0) < v:
                    waited[ename][k] = v
            ops[ename] = []

    def finish(self):
        if self.pes is not None:
            self.emit(final=True)
            self.pes.close()
            self.pes = None
        else:
            self.emit(final=True)
        self.es.close()
        return self.nc


def run_prog(prog, in_maps):
    nc = prog.finish()
    res = run_bass_kernel_spmd(nc, in_maps, core_ids=list(range(len(in_maps))))
    return res.results


def load_weight_bf16(p, wdram, wb, K, N, stage, col0=0, engs=("dve", "act")):
    SW = stage[0].h.shape[1]
    i = 0
    for kc in range(K // 128):
        for c0 in range(0, N, SW):
            w = min(SW, N - c0)
            st = stage[i % len(stage)]
            p.dma(st[:, 0:w], wdram[kc * 128:(kc + 1) * 128, c0:c0 + w])
            p.copy(wb[:, kc, col0 + c0:col0 + c0 + w], st[:, 0:w], eng=engs[i % len(engs)])
            i += 1


def rms_to_fm(p, xt, hnT, tslot, g_rep, ident, scr, xs, pT, ssq, rstd):
    p.act(scr[:, :], xt[:, :], AF.Square, accum=ssq[:, 0:1])
    p.act(rstd[:, 0:1], ssq[:, 0:1], AF.Sqrt, bias=EPS, scale=1.0 / D)
    p.recip(rstd[:, 0:1], rstd[:, 0:1])
    p.ts(xs[:, :], xt[:, :], rstd[:, 0:1], None, ALU.mult)
    for c in range(8):
        p.tr(pT[:, c * 128:(c + 1) * 128], xs[:, c * 128:(c + 1) * 128], ident[:, :])
    p.tt(hnT[:, 0:8, tslot * 128:(tslot + 1) * 128],
         pT.v(pT.h[:, :].rearrange("p (c t) -> p c t", c=8)),
         g_rep.v(g_rep.h[:, :].rearrange("p (c t) -> p c t", c=8)), ALU.mult)


def phase_proj(p, NT, CF, fm_dt, tm_segs, h, W, g_rep_d, ident_d, zfm, ztm, G=512, pre=None, is_out=False):
    CT = sum(w for _, w, _ in tm_segs)
    N = CF + CT

    wb = p.sb("wb", [128, 8, N], BF16)
    stage = [p.sb("stage%d" % i, [128, 1024], F32) for i in range(2)]
    g_rep = p.sb("g_rep_sb", [128, 1024], F32)
    ident = p.sb("ident_sb", [128, 128], BF16)
    xt = [p.sb("xt%d" % i, [128, D], F32) for i in range(2)]
    scr = p.sb("scr", [128, D], BF16)
    xs = [p.sb("xs%d" % i, [128, D], BF16) for i in range(2)]
    ssq = [p.sb("ssq%d" % i, [128, 1], F32) for i in range(2)]
    rstd = [p.sb("rstd%d" % i, [128, 1], F32) for i in range(2)]
    if pre is None:
        hnT = [p.sb("hnT%d" % i, [128, 8, G], BF16) for i in range(2)]
    else:
        hnT = [None, None]
        hh = p.sb("hh", [128, 8, 4096], BF16)
    ofm = [p.sb("ofm%d" % i, [128, G], fm_dt) for i in range(3)]
    otm = [[p.sb("otm_%s%d" % (nm, i), [128, w], dt) for i in range(2)] for nm, w, dt in tm_segs]
    pT = [p.ps("pT%d" % i, [128, 1024], BF16) for i in range(2)]
    pm = [p.ps("pm%d" % i, [128, 512], F32) for i in range(4)]

    if pre is None:
        p.dma(g_rep[:, :], g_rep_d[:, :])
        p.dma(ident[:, :], ident_d[:, :])
    load_weight_bf16(p, W, wb, D, N, stage)

    TPG = G // 128
    it = 0
    k_pm = 0
    k_of = 0
    for gi in range(NT // G):
        hb = hnT[gi % 2]
        if pre is not None:
            G3, basefn = pre
            half, c0 = (gi * G) // 4096, (gi * G) % 4096
            if c0 == 0:
                p.dma(hh[:, :, :],
                      G3.v((lambda e, half=half: G3.h[bass.ds(basefn(e) + half * 1024, 1024), :]
                            .rearrange("(c p) n -> p c n", p=128))))
            hs = lambda c, a, b_, c0=c0: hh[:, c, c0 + a:c0 + b_]
        else:
            hs = lambda c, a, b_, hb=hb: hb[:, c, a:b_]
            for ti in range(TPG):
                t0 = gi * G + ti * 128
                b = it % 2
                p.dma(xt[b][:, :], h[t0:t0 + 128, :])
                rms_to_fm(p, xt[b], hb, ti, g_rep, ident, scr, xs[b], pT[b], ssq[b], rstd[b])
                it += 1
        for m0 in range(0, CF, 128):
            mw = min(128, CF - m0)
            ps_ = pm[k_pm % 4]
            k_pm += 1
            for c in range(8):
                p.mm(ps_[0:mw, 0:G], wb[:, c, m0:m0 + mw], hs(c, 0, G), start=(c == 0), stop=(c == 7))
            o = ofm[k_of % 3]
            if k_of % 2 == 0:
                p.copy(o[0:mw, :], ps_[0:mw, 0:G], eng="act")
            else:
                p.copy(o[0:mw, :], ps_[0:mw, 0:G], eng="dve")
            k_of += 1
            p.dma(zfm[m0:m0 + mw, gi * G:(gi + 1) * G], o[0:mw, :], is_out=is_out)
        for ti in range(TPG):
            t0 = gi * G + ti * 128
            off = CF
            for si, (nm, w, dt) in enumerate(tm_segs):
                o = otm[si][ti % 2]
                for c0 in range(0, w, 512):
                    cw = min(512, w - c0)
                    ps_ = pm[k_pm % 4]
                    k_pm += 1
                    for c in range(8):
                        p.mm(ps_[:, 0:cw], hs(c, ti * 128, (ti + 1) * 128),
                             wb[:, c, off + c0:off + c0 + cw], start=(c == 0), stop=(c == 7))
                    if k_of % 2 == 0:
                        p.copy(o[:, c0:c0 + cw], ps_[:, 0:cw], eng="act")
                    else:
                        p.copy(o[:, c0:c0 + cw], ps_[:, 0:cw], eng="dve")
                    k_of += 1
                p.dma(ztm[si][t0:t0 + 128, :], o[:, :], is_out=is_out)
                off += w


def phase_outproj(p, NT, x, Gt, basefn, Wo, h1, G=512):
    wb = p.sb("wb", [128, 8, D], BF16)
    stage = [p.sb("stage%d" % i, [128, 1024], F32) for i in range(2)]
    ob = p.sb("ot", [128, 8, NT], BF16)
    xt = [p.sb("xt%d" % i, [128, D], F32) for i in range(3)]
    pm = [p.ps("pm%d" % i, [128, 512], F32) for i in range(4)]
    p.dma([ob[:, r * 4:(r + 1) * 4, :] for r in range(2)],
          [Gt.v((lambda e, r=r: Gt.h[bass.ds(basefn(e) + r * 1024, 512), :].rearrange("(c p) n -> p c n", p=128)))
           for r in range(2)])
    load_weight_bf16(p, Wo, wb, D, D, stage)
    k = 0
    it = 0
    for ti in range(NT // 128):
        t0 = ti * 128
        xb = xt[it % 3]
        it += 1
        p.dma(xb[:, :], x[t0:t0 + 128, :])
        for half in range(2):
            ps_ = pm[k % 4]
            k += 1
            for c in range(8):
                p.mm(ps_[:, :], ob[:, c, t0:t0 + 128], wb[:, c, half * 512:(half + 1) * 512],
                     start=(c == 0), stop=(c == 7))
            p.tt(xb[:, half * 512:(half + 1) * 512], ps_[:, :], xb[:, half * 512:(half + 1) * 512], ALU.add)
        p.dma(h1[t0:t0 + 128, :], xb[:, :], is_out=False)


def phase_ffn(p, NT, h1, Wg, Wu, Wd, g_rep_d, ident_d, h2, final=False, gfin_d=None, nxt=None, G=256):
    NF = FFN // 128
    wg = p.sb("wg", [128, 8, FFN], BF16)
    wu = p.sb("wu", [128, 8, FFN], BF16)
    wd = p.sb("wd", [128, NF, D], BF16)
    stage = [p.sb("stage%d" % i, [128, 1024], F32) for i in range(2)]
    g_rep = p.sb("g_rep_sb", [128, 1024], F32)
    ident = p.sb("ident_sb", [128, 128], BF16)
    TPG = G // 128
    xt = [p.sb("xt%d" % i, [128, D], F32) for i in range(TPG + 1)]
    scr = p.sb("scr", [128, D], BF16)
    xs = [p.sb("xs%d" % i, [128, D], BF16) for i in range(2)]
    ssq = [p.sb("ssq%d" % i, [128, 1], F32) for i in range(2)]
    rstd = [p.sb("rstd%d" % i, [128, 1], F32) for i in range(2)]
    hnT = [p.sb("hnT%d" % i, [128, 8, G], BF16) for i in range(2)]
    aT = p.sb("aT", [128, NF, G], BF16)
    sil = [p.sb("sil%d" % i, [128, G], F32) for i in range(2)]
    if final:
        gfin = p.sb("gfin_sb", [128, D], F32)
        ssq2 = p.sb("ssq2", [128, 1], F32)
        rstd2 = p.sb("rstd2", [128, 1], F32)
    if nxt is not None:
        g2_rep = p.sb("g2_rep_sb", [128, 1024], F32)
        p.dma(g2_rep[:, :], nxt[0][:, :])
        hn2 = [p.sb("hn2_%d" % i, [128, 8, 128], BF16) for i in range(2)]
    pT = [p.ps("pT%d" % i, [128, 1024], BF16) for i in range(2)]
    pg = [p.ps("pg%d" % i, [128, 512], F32) for i in range(2)]
    pu = [p.ps("pu%d" % i, [128, 512], F32) for i in range(2)]
    pd = [p.ps("pd%d" % i, [128, 512], F32) for i in range(2)]

    p.dma(g_rep[:, :], g_rep_d[:, :])
    p.dma(ident[:, :], ident_d[:, :])
    if final:
        p.dma(gfin[:, :], gfin_d[:, :])
    load_weight_bf16(p, Wg, wg, D, FFN, stage)
    load_weight_bf16(p, Wu, wu, D, FFN, stage)
    load_weight_bf16(p, Wd, wd, FFN, D, stage)

    it = 0
    kf = 0
    kd = 0
    for gi in range(NT // G):
        hb = hnT[gi % 2]
        xts = []
        for ti in range(TPG):
            t0 = gi * G + ti * 128
            xb = xt[it % (TPG + 1)]
            b = it % 2
            it += 1
            xts.append(xb)
            p.dma(xb[:, :], h1[t0:t0 + 128, :])
            rms_to_fm(p, xb, hb, ti, g_rep, ident, scr, xs[b], pT[b], ssq[b], rstd[b])
        for f in range(NF):
            b = kf % 2
            kf += 1
            for c in range(8):
                p.mm(pg[b][:, 0:G], wg[:, c, f * 128:(f + 1) * 128], hb[:, c, :], start=(c == 0), stop=(c == 7))
            for c in range(8):
                p.mm(pu[b][:, 0:G], wu[:, c, f * 128:(f + 1) * 128], hb[:, c, :], start=(c == 0), stop=(c == 7))
            p.act(sil[b][:, :], pg[b][:, 0:G], AF.Silu)
            p.tt(aT[:, f, :], sil[b][:, :], pu[b][:, 0:G], ALU.mult)
        for ti in range(TPG):
            t0 = gi * G + ti * 128
            xb = xts[ti]
            for half in range(2):
                ps_ = pd[kd % 2]
                kd += 1
                for f in range(NF):
                    p.mm(ps_[:, :], aT[:, f, ti * 128:(ti + 1) * 128], wd[:, f, half * 512:(half + 1) * 512],
                         start=(f == 0), stop=(f == NF - 1))
                p.tt(xb[:, half * 512:(half + 1) * 512], ps_[:, :], xb[:, half * 512:(half + 1) * 512], ALU.add)
            if final:
                p.act(scr[:, :], xb[:, :], AF.Square, accum=ssq2[:, 0:1])
                p.act(rstd2[:, 0:1], ssq2[:, 0:1], AF.Sqrt, bias=EPS, scale=1.0 / D)
                p.recip(rstd2[:, 0:1], rstd2[:, 0:1])
                p.stt(xb[:, :], xb[:, :], rstd2[:, 0:1], gfin[:, :], ALU.mult, ALU.mult)
            p.dma(h2[t0:t0 + 128, :], xb[:, :], is_out=final)
            if nxt is not None:
                b = it % 2
                it += 1
                rms_to_fm(p, xb, hn2[b], 0, g2_rep, ident, scr, xs[b], pT[b], ssq[b], rstd[b])
                p.dma([nxt[1][c * 128:(c + 1) * 128, t0:t0 + 128] for c in range(8)],
                      [hn2[b][:, c, :] for c in range(8)])


NEG = -30000.0
import os
HAMTEST = int(os.environ.get('HAMTEST', '0'))
NB = SEQ // 128
FM_QA, FM_KC, FM_VC, FM_KS, FM_KW, FM_QB, FM_KB = 0, 256, 320, 384, 448, 512, 768


def attn_consts():
    k = np.arange(128)[:, None]
    q = (np.arange(512) % 128)[None, :]
    c = {}
    c["ident"] = np.eye(128).astype(NPBF)
    c["mdiag"] = np.where(k <= q, 0.0, NEG).astype(NPBF)
    c["mfar"] = np.where(k > q, 0.0, NEG).astype(NPBF)
    cm = np.zeros((128, 17, 512), np.float32)
    for dl in range(17):
        cm[:, dl, :] = np.where(16 * k + 31 - q <= 128 * dl, 0.0, NEG)
    c["cmpmask"] = cm.reshape(128, 17 * 512).astype(NPBF)
    s = np.arange(64)[:, None]
    kk = np.arange(SEQ)[None, :]
    c["E2"] = ((kk // 64) % 64 == s).astype(np.float32).astype(NPBF)
    cs = np.arange(512)[:, None] * 16
    ss = np.arange(128)[None, :] * 64
    sm = ((cs < ss + 64) & (cs + 32 > ss)).astype(np.float32)
    sm[511, :] = 0.0
    c["selmap"] = sm.reshape(4, 128, 128).transpose(1, 0, 2).reshape(128, 512).astype(NPBF)
    ql = np.arange(128)[:, None]
    sp = np.arange(256)[None, :] - 126
    cur = ql // 64
    c["tb1"] = (sp < cur - 1).astype(np.float32)
    c["tb2"] = np.where(sp > cur, -1e30, np.where(sp >= cur - 1, 1e9, 0.0)).astype(np.float32)
    c["tb3"] = (sp <= cur).astype(np.float32)
    cv = np.ones((128, 4), np.float32)
    cv[127, 3] = 0.0
    c["cvalid"] = cv
    return c


ATTN_CONST_SPECS = (("ident", [128, 128], BF16), ("mdiag", [128, 512], BF16), ("mfar", [128, 512], BF16),
                    ("cmpmask", [128, 17 * 512], BF16), ("E2", [64, SEQ], BF16), ("selmap", [128, 512], BF16),
                    ("tb1", [128, 256], F32), ("tb2", [128, 256], F32), ("tb3", [128, 256], F32),
                    ("cvalid", [128, 4], F32))


def phase_attn(p, fm, tmv, gts, ai, xi, nblk=NB):
    T_ = SEQ
    gbias_d, sinks_d, posk_d, posv_d = ai["gbias"], ai["sinks"], ai["posk"], ai["posv"]
    w1k_d, w1v_d, w2k_d, w2v_d = ai["w1k"], ai["w1v"], ai["w2k"], ai["w2v"]
    cd = {}
    for nm, shp, dt in ATTN_CONST_SPECS:
        if nm == "E2":
            continue
        d_ = ai[nm]
        s_ = p.sb(nm + "_sb", shp, dt)
        p.dma(s_[:, :], d_[:, :])
        cd[nm] = s_
    ident, mdiag, mfar, cmpmask, selmap = (cd[k] for k in ("ident", "mdiag", "mfar", "cmpmask", "selmap"))
    tb1, tb2, tb3, cvalid = cd["tb1"], cd["tb2"], cd["tb3"], cd["cvalid"]

    ksT = p.sb("ksT", [128, T_], BF16)
    p.dma([ksT[64:128, i * 2048:(i + 1) * 2048] for i in range(4)],
          [ai["E2"][:, i * 2048:(i + 1) * 2048] for i in range(4)])
    kwT = p.sb("kwT", [128, T_], BF16)
    kbT = p.sb("kbT", [128, T_], BF16)
    p.memset(kwT[64:128, :], 0.0, eng="pool")
    p.memset(kbT[64:128, :], 0.0, eng="pool")
    kcin = p.sb("kcin", [64, T_], BF16)
    vcin = p.sb("vcin", [64, T_], BF16)
    for dst, r0 in ((ksT, FM_KS), (kwT, FM_KW), (kbT, FM_KB), (kcin, FM_KC), (vcin, FM_VC)):
        p.dma([dst[0:64, i * 2048:(i + 1) * 2048] for i in range(4)],
              [fm[r0:r0 + 64, i * 2048:(i + 1) * 2048] for i in range(4)])
    vs1 = p.sb("vs1", [128, NB, 65], BF16)
    vw1 = p.sb("vw1", [128, NB, 65], BF16)
    vb1 = p.sb("vb1", [128, NB, 65], BF16)
    for i, vt in enumerate((vs1, vw1, vb1)):
        p.memset(vt[:, :, :], 1.0, eng="pool")
        src = tmv.h[:, i * 64:(i + 1) * 64].rearrange("(j p) d -> p j d", p=128)
        p.dma([vt[:, j * 8:(j + 1) * 8, 0:64] for j in range(8)],
              [tmv.v(src[:, j * 8:(j + 1) * 8, :]) for j in range(8)])
    gate = p.sb("gate", [128, NB * 12], F32)
    gb = p.sb("gb", [128, NB * 12], F32)
    p.dma(gate.v(gate.h[:, :].rearrange("p (j c) -> p j c", c=12)),
          gts.v(gts.h[:, :].rearrange("(j p) c -> p j c", p=128)))
    p.dma(gb[:, :], gbias_d[:, :])
    p.tt(gate[:, :], gate[:, :], gb[:, :], ALU.add)
    p.act(gate[:, :], gate[:, :], AF.Exp, scale=-1.0)
    p.ts(gate[:, :], gate[:, :], 1.0, None, ALU.add)
    p.recip(gate[:, :], gate[:, :])
    gate3 = gate.h[:, :].rearrange("p (j h c) -> p j h c", h=4, c=3)
    esink = p.sb("esink", [128, 4], F32)
    p.dma(esink[:, :], sinks_d[:, :])
    p.act(esink[:, :], esink[:, :], AF.Exp)

    S = [p.ps("S%d" % i, [128, 512], F32) for i in range(3)]
    OcT = p.ps("OcT", [128, 512], F32)
    U = p.ps("U", [128, 512], F32)
    OsT = p.ps("OsT", [128, 512], F32)
    OwT = p.ps("OwbT", [128, 512], F32)
    ObT = OwT
    X = p.ps("X", [128, 512], F32)
    Xb = X.h.bitcast(BF16)
    identf = p.sb("identf", [128, 128], F32)
    p.copy(identf[:, :], ident[:, :])
    otf = [p.sb("otf%d" % i, [65, 512], F32) for i in range(4)]
    st_of = [0]

    kcT = p.sb("kcT", [128, 512], BF16)
    vc1 = p.sb("vc1", [128, 4, 65], BF16)
    stg = p.sb("stg", [64, 2048], F32)
    w1b = p.sb("w1b", [64, 2048], BF16)
    w2s = p.sb("w2s", [64, 64], F32)
    w2b = p.sb("w2b", [64, 64], BF16)
    poss = p.sb("poss", [64, 32], F32)
    posb = p.sb("posb", [64, 32], BF16)
    bcol = p.sb("bcol", [64, 1], F32)
    h1T = p.sb("h1T", [64, 512], BF16)
    p.memset(h1T[:, :], 0.0)
    p.memset(kcT[:, :], 0.0)
    p.memset(vc1[:, :, :], 0.0)
    for which, (w1d, w2d, posd, src) in enumerate(((w1k_d, w2k_d, posk_d, kcin), (w1v_d, w2v_d, posv_d, vcin))):
        p.dma(stg[:, :], w1d[:, :])
        p.copy(w1b[:, :], stg[:, :])
        p.dma(w2s[:, :], w2d[:, :])
        p.copy(w2b[:, :], w2s[:, :])
        p.dma(poss[:, :], posd[:, :])
        p.copy(posb[:, :], poss[:, :])
        for l in range(32):
            p.mm(S[0][0:64, 0:1], w1b[:, l * 64:(l + 1) * 64], posb[:, l:l + 1], start=(l == 0), stop=(l == 31))
        p.copy(bcol[:, :], S[0][0:64, 0:1])
        sv = src.h[:, :].rearrange("p (i r) -> p i r", r=16)
        for l in range(32):
            rhs = sv[:, 0:511, l] if l < 16 else sv[:, 1:512, l - 16]
            p.mm(S[1][0:64, 0:511], w1b[:, l * 64:(l + 1) * 64], src.v(rhs), start=(l == 0), stop=(l == 31))
        p.act(h1T[:, 0:511], S[1][0:64, 0:511], AF.Silu, bias=bcol[:, 0:1])
        if which == 0:
            p.mm(S[0][0:64, 0:511], w2b[:, :], h1T[:, 0:511])
            p.copy(kcT[0:64, 0:511], S[0][0:64, 0:511])
        else:
            for m in range(4):
                p.mm(S[0][:, m * 64:(m + 1) * 64], h1T[:, m * 128:(m + 1) * 128], w2b[:, :])
            p.copy(vc1[:, :, 0:64], S[0].v(S[0].h[:, 0:256].rearrange("p (m d) -> p m d", m=4)))
            p.copy(vc1[:, :, 64], cvalid[:, :])

    qa = [p.sb("qa%d" % i, [128, 512], BF16) for i in range(3)]
    qa1 = [p.sb("qa1_%d" % i, [128, 512], BF16) for i in range(3)]
    nmsw = p.sb("nmsw", [128, 128], BF16)
    for t_ in qa + qa1:
        p.memset(t_[64:128, :], 0.0)
    qb = [p.sb("qb%d" % i, [128, 512], BF16) for i in range(2)]
    for t_ in qb:
        p.memset(t_[64:128, :], 0.0)
    Pb = [p.sb("P%d" % i, [128, 512], BF16) for i in range(4)]
    nmT = [p.sb("nmT%d" % i, [128, 512], BF16) for i in range(2)]
    oacc = [p.sb("oacc%d" % i, [128, 512], F32) for i in range(2)]
    obf = p.sb("obf", [128, 512], BF16)
    oTs = [p.sb("oTs%d" % i, [128, 512], BF16) for i in range(2)]
    imp = p.sb("imp", [128, 128], F32)
    score = p.sb("score", [128, 128], F32)
    sc2 = p.sb("sc2", [128, 128], F32)
    m8 = p.sb("m8", [128, 8], F32)
    thr = p.sb("thr", [128, 1], F32)
    sel = p.sb("sel", [128, 128], F32)
    nmb = p.sb("nmb", [128, 128], BF16)
    lt = [p.sb("lt%d" % i, [128, 4], F32) for i in range(4)]
    wg_ = [p.sb("wgt%d" % i, [128, 4], F32) for i in range(4)]
    st = {"S": 0, "P": 0}

    def load_q(n):
        cs = slice(n * 128, (n + 1) * 128)
        for a in ([qa[n % 3]] if n < 32 else [qa[n % 3], qa1[n % 3]]):
            p.dma(a.v(a.h[0:64, :].rearrange("d (h t) -> d h t", h=4)),
                  fm.v(fm.h[FM_QA:FM_QA + 256, cs].rearrange("(h d) t -> d h t", d=64)))
        b = qb[n % 2]
        p.dma(b.v(b.h[0:64, :].rearrange("d (h t) -> d h t", h=4)),
              fm.v(fm.h[FM_QB:FM_QB + 256, cs].rearrange("(h d) t -> d h t", d=64)))

    pend = []

    def defer(k, fn):
        pend.append([k, fn])

    def tick():
        for e_ in pend:
            e_[0] -= 1
        while pend and pend[0][0] <= 0:
            pend.pop(0)[1]()

    def flush_pending():
        while pend:
            pend.pop(0)[1]()

    def branch(specs, OT, extra=None):
        nt = len(specs)
        banks = {}

        def emitS(i):
            ps_ = S[st["S"] % 3]
            st["S"] += 1
            banks[i] = ps_
            mms = specs[i][0]
            for idx, (l, r) in enumerate(mms):
                p.mm(ps_[:, :], l, r, start=(idx == 0), stop=(idx == len(mms) - 1))
        emitS(0)
        if nt > 1:
            emitS(1)
        tick()
        for i in range(nt):
            if i + 2 < nt:
                emitS(i + 2)
            P_ = Pb[st["P"] % 4]
            st["P"] += 1
            p.act(P_[:, :], banks[i][:, :], AF.Exp, scale=0.125)
            p.mm(OT[0:65, :], specs[i][1], P_[:, :], start=(i == 0), stop=(i == nt - 1))
            if extra is not None:
                extra(P_, i, i == 0, i == nt - 1)
            tick()

    def to_token_major(OT, then, k2=4):
        if sum(1 for e_ in pend if e_[1].__name__ == "later") >= 3:
            flush_pending()
        of = otf[st_of[0] % 4]
        st_of[0] += 1
        p.copy(of[:, :], OT[0:65, :], eng="dve")

        def later():
            for h in range(4):
                p.tr(X[:, 128 + h * 65:128 + (h + 1) * 65], of[:, h * 128:(h + 1) * 128], identf[0:65, 0:65])
            r = then()
            if r is not None:
                pend.insert(0, [k2, r])
        defer(1, later)

    class OV:
        tile = X
        h = None

        def __getitem__(self, idx):
            rs, cs = idx
            return X[rs, slice(128 + cs.start, 128 + cs.stop)]
    Otm = OV()

    def ovw(O):
        return X.v(X.h[:, 128:388].rearrange("p (h e) -> p h e", e=65))

    def norm_weights(O, k, gate_j, n):
        if gate_j is None:
            p.tt(lt[k][:, :], X.v(ovw(O).ap[:, :, 64]), esink[:, :], ALU.add)
        else:
            p.ts(lt[k][:, :], X.v(ovw(O).ap[:, :, 64]), 1e-30, None, ALU.max)
        p.recip(lt[k][:, :], lt[k][:, :])
        if gate_j is None:
            return lt[k]
        p.tt(wg_[k][:, :], lt[k][:, :], gate.v(gate3[:, n, :, gate_j]), ALU.mult)
        return wg_[k]

    def cmp_and_topk(n):
        a = qa[n % 3]
        ntc = min(4, n // 16 + 1)
        specs = []
        for m in range(ntc):
            mms = [(kcT[:, m * 128:(m + 1) * 128], a[:, :])]
            dl = n - 16 * m
            if dl <= 16:
                mms.append((ident[:, :], cmpmask[:, dl * 512:(dl + 1) * 512]))
            specs.append((mms, vc1[:, m, :]))

        def extra(P_, i, first, last):
            for h in range(4):
                p.mm(U[:, h * 128:(h + 1) * 128], P_[:, h * 128:(h + 1) * 128], selmap[:, i * 128:(i + 1) * 128],
                     start=(first and h == 0), stop=last, skip=True)
        branch(specs, OcT, extra)

        def cont():
            w = norm_weights(Otm, 0, 0, n)
            oa = oacc[n % 2]
            for h in range(4):
                p.ts(oa[:, h * 64:(h + 1) * 64], Otm[:, h * 65:h * 65 + 64], w[:, h:h + 1], None, ALU.mult)
            rl = lt[0]
            p.ts(imp[:, :], U[:, 0:128], rl[:, 0:1], None, ALU.mult)
            for h in range(1, 4):
                p.stt(imp[:, :], U[:, h * 128:(h + 1) * 128], rl[:, h:h + 1], imp[:, :], ALU.mult, ALU.add)
            u0 = 126 - 2 * n
            p.tt(score[:, :], imp[:, :], tb1[:, u0:u0 + 128], ALU.mult)
            p.tt(score[:, :], score[:, :], tb2[:, u0:u0 + 128], ALU.add)
            p.memset(score[:, 0:1], 1e9)
            so, s2o, m8o = score.h[:, :], sc2.h[:, :], m8.h[:, :]
            p.op("dve", lambda e: e.max(out=m8o, in_=so), [m8.all()], [score.all()])
            p.op("dve", lambda e: e.match_replace(out=s2o, in_to_replace=m8o, in_values=so, imm_value=-3e38),
                 [sc2.all()], [m8.all(), score.all()])
            p.op("dve", lambda e: e.max(out=m8o, in_=s2o), [m8.all()], [sc2.all()])
            tho = thr.h[:, :]
            p.op("dve", lambda e: e.tensor_reduce(tho, m8o, AX.X, ALU.min), [thr.all()], [m8.all()])
            p.stt(sel[:, :], score[:, :], thr[:, 0:1], tb3[:, u0:u0 + 128], ALU.is_ge, ALU.mult)
            p.ts(nmsw[:, 64:128], sel[:, 0:64], 1.0, -NEG, ALU.subtract, ALU.mult)
            p.ts(nmsw[:, 0:64], sel[:, 64:128], 1.0, -NEG, ALU.subtract, ALU.mult)
            if n >= 32:
                p.ts(nmb[:, :], sel[:, :], 1.0, -NEG, ALU.subtract, ALU.mult)
            return stage2

        def stage2():
            p.tr(X.v(Xb[:, 0:128]), nmsw[:, :], ident[:, :])
            for h in range(4):
                p.copy(a[64:128, h * 128:(h + 1) * 128], X.v(Xb[64:128, 0:128]), eng="dve")
            if n >= 32:
                p.tr(X.v(Xb[:, 128:256]), nmb[:, :], ident[:, :])
                a1 = qa1[n % 3]
                for h in range(4):
                    p.copy(a1[64:128, h * 128:(h + 1) * 128], X.v(Xb[64:128, 128:256]), eng="dve")
        to_token_major(OcT, cont, k2=14)

    def rest(n):
        a = qa[n % 3]
        a1 = qa1[n % 3]
        b = qb[n % 2]
        oa = oacc[n % 2]
        ks_ = lambda j: slice(j * 128, (j + 1) * 128)
        specs = []
        for j in range(max(0, n - 4), n + 1):
            mms = [(kwT[:, ks_(j)], a[:, :])]
            if j == n:
                mms.append((ident[:, :], mdiag[:, :]))
            if j == n - 4:
                mms.append((ident[:, :], mfar[:, :]))
            specs.append((mms, vw1[:, j, :]))
        branch(specs, OwT)

        def cont_w():
            w2_ = norm_weights(Otm, 2, 2, n)
            for h in range(4):
                p.stt(oa[:, h * 64:(h + 1) * 64], Otm[:, h * 65:h * 65 + 64], w2_[:, h:h + 1],
                      oa[:, h * 64:(h + 1) * 64], ALU.mult, ALU.add)
        to_token_major(OwT, cont_w)
        if any(e_[1].__name__ in ("later", "stage2") for e_ in pend):
            flush_pending()
        specs = []
        for j in range(n + 1):
            mms = [(ksT[:, ks_(j)], (a if j < 32 else a1)[:, :])]
            if j == n:
                mms.append((ident[:, :], mdiag[:, :]))
            specs.append((mms, vs1[:, j, :]))
        branch(specs, OsT)

        def cont_s():
            w1_ = norm_weights(Otm, 1, 1, n)
            for h in range(4):
                p.stt(obf[:, h * 64:(h + 1) * 64], Otm[:, h * 65:h * 65 + 64], w1_[:, h:h + 1],
                      oa[:, h * 64:(h + 1) * 64], ALU.mult, ALU.add)
        to_token_major(OsT, cont_s)
        specs = []
        for j in range(max(0, n - 1), n + 1):
            mms = [(kbT[:, ks_(j)], b[:, :])]
            if j == n:
                mms.append((ident[:, :], mdiag[:, :]))
            if j == n - 1:
                mms.append((ident[:, :], mfar[:, :]))
            specs.append((mms, vb1[:, j, :]))
        branch(specs, ObT)

        def cont_b():
            w3_ = norm_weights(Otm, 3, None, n)
            for h in range(4):
                p.ts(obf[:, 256 + h * 64:256 + (h + 1) * 64], Otm[:, h * 65:h * 65 + 64], w3_[:, h:h + 1], None, ALU.mult)
            return stage_out

        def stage_out():
            for c in range(4):
                p.tr(X.v(Xb[:, 256 + c * 128:256 + (c + 1) * 128]), obf[:, c * 128:(c + 1) * 128], ident[:, :])
            ot = oTs[n % 2]
            p.copy(ot[:, :], X.v(Xb[:, 256:768]), eng="dve")
            r0 = (n // 32) * 512
            cc = (n % 32) * 128
            p.dma([xi[r0 + c * 128:r0 + (c + 1) * 128, cc:cc + 128] for c in range(4)],
                  [ot[:, c * 128:(c + 1) * 128] for c in range(4)])
        to_token_major(ObT, cont_b, k2=5)

    load_q(0)
    cmp_and_topk(0)
    for n in range(nblk):
        if n + 1 < nblk:
            load_q(n + 1)
            cmp_and_topk(n + 1)
        rest(n)
    flush_pending()


def ab_perm():
    off = dict(qa=0, kc=512, vc=640, ks=768, vs=896, kw=1024, vw=1152, g=1280, qb=1304, kb=1816, vb=1944)
    fmc = []
    for g in range(2):
        fmc += list(range(off["qa"] + g * 256, off["qa"] + (g + 1) * 256))
        for nm in ("kc", "vc", "ks", "kw"):
            fmc += list(range(off[nm] + g * 64, off[nm] + (g + 1) * 64))
        fmc += list(range(off["qb"] + g * 256, off["qb"] + (g + 1) * 256))
        fmc += list(range(off["kb"] + g * 64, off["kb"] + (g + 1) * 64))
    tmc = []
    for g in range(2):
        for nm in ("vs", "vw", "vb"):
            tmc += list(range(off[nm] + g * 64, off[nm] + (g + 1) * 64))
    gc = list(range(1280, 1304))
    return fmc, tmc, gc


def g_rep_of(g):
    return np.ascontiguousarray(
        np.repeat(np.asarray(g, np.float32).reshape(8, 128).T[:, :, None], 128, axis=2).reshape(128, 1024))


def attn_inputs(g, fm, tmv, gts, gate_bias, sinks, pos_k, pos_v, w1k, w2k, w1v, w2v, consts):
    d = dict(consts)
    d["fm"] = None if fm is None else np.ascontiguousarray(fm)
    d["tmv"] = None if tmv is None else np.ascontiguousarray(tmv)
    d["gts"] = None if gts is None else np.ascontiguousarray(gts)
    d["gbias"] = np.ascontiguousarray(np.tile(np.asarray(gate_bias[g * 12:(g + 1) * 12], np.float32)[None, :], (128, NB)))
    d["sinks"] = np.ascontiguousarray(np.broadcast_to(np.asarray(sinks[g * 4:(g + 1) * 4], np.float32)[None, :], (128, 4)))
    d["posk"] = np.ascontiguousarray(np.asarray(pos_k, np.float32).T)
    d["posv"] = np.ascontiguousarray(np.asarray(pos_v, np.float32).T)
    r1 = lambda w: np.ascontiguousarray(np.asarray(w, np.float32).reshape(32, 64, 64).transpose(1, 0, 2).reshape(64, 2048))
    d["w1k"] = r1(w1k)
    d["w1v"] = r1(w1v)
    d["w2k"] = np.ascontiguousarray(np.asarray(w2k, np.float32))
    d["w2v"] = np.ascontiguousarray(np.asarray(w2v, np.float32))
    return d


def mlstm_consts():
    s = np.arange(128)[:, None]
    t = np.arange(128)[None, :]
    c = {}
    c["ident"] = np.eye(128).astype(NPBF)
    c["tri"] = (s <= t).astype(np.float32)
    c["ones"] = np.ones((128, 128), np.float32)
    return c


def phase_mlstm(p, qkfm, vtm, ogtm, gtm, mi, xi, nch=SEQ // 128, dbg=99):
    T_ = SEQ
    NCH = SEQ // 128
    cd = {}
    for nm, shp, dt in (("ident", [128, 128], BF16), ("tri", [128, 128], F32), ("ones", [128, 128], F32),
                        ("cw", [128, 16], F32), ("cb", [128, 4], F32), ("ibf", [128, NCH * 2], F32),
                        ("fbf", [128, NCH * 2], F32)):
        if True:
            d_ = mi[nm]
        s_ = p.sb(nm + "_sb", shp, dt)
        p.dma(s_[:, :], d_[:, :])
        cd[nm] = s_
    ident, tri, ones, cw, cb, ibf, fbf = (cd[k] for k in ("ident", "tri", "ones", "cw", "cb", "ibf", "fbf"))

    A = [p.ps("A%d" % i, [128, 512], F32) for i in range(2)]
    B = [p.ps("B%d" % i, [128, 512], F32) for i in range(2)]
    Cn = [p.ps("Cn%d" % i, [128, 512], F32) for i in range(2)]
    pTk = p.ps("pTk", [128, 1024], BF16)
    pTh = p.ps("pTh", [128, 1024], BF16)

    gt = p.sb("gt", [128, NCH * 4], F32)
    p.dma(gt.v(gt.h[:, :].rearrange("p (j c) -> p j c", c=4)),
          gtm.v(gtm.h[:, :].rearrange("(j p) c -> p j c", p=128)))
    gt3 = gt.h[:, :].rearrange("p (j c) -> p j c", c=4)
    icb = p.sb("icb", [128, NCH * 2], F32)
    sp = p.sb("sp", [128, NCH * 2], F32)
    v3 = lambda t: t.h[:, :].rearrange("p (j c) -> p j c", c=2)
    p.tt(icb.v(v3(icb)), gt.v(gt3[:, :, 0:2]), ibf.v(v3(ibf)), ALU.add)
    p.tt(sp.v(v3(sp)), gt.v(gt3[:, :, 2:4]), fbf.v(v3(fbf)), ALU.add)
    if dbg == -1:
        return
    p.act(sp[:, :], sp[:, :], AF.Exp, scale=-1.0)
    p.act(sp[:, :], sp[:, :], AF.Ln, bias=1.0)
    NC2 = NCH * 2
    if dbg == -2:
        return
    p.mm(A[0][:, 0:NC2], tri[:, :], sp[:, :])
    p.mm(A[1][:, 0:NC2], ones[:, :], sp[:, :])
    eb = p.sb("eb", [128, NC2], F32)
    eu = p.sb("eu", [128, NC2], F32)
    ebl = p.sb("ebl", [128, NC2], F32)
    if dbg == -3:
        return
    p.act(eb[:, :], A[0][:, 0:NC2], AF.Exp, scale=-1.0)
    if dbg == -4:
        return
    p.tt(eu[:, :], icb[:, :], A[0][:, 0:NC2], ALU.add)
    if dbg == -5:
        return
    p.act(eu[:, :], eu[:, :], AF.Exp)
    if dbg == -6:
        return
    p.act(ebl[:, :], A[1][:, 0:NC2], AF.Exp, scale=-1.0)
    if dbg == -7:
        return
    eub = p.sb("eub", [128, NC2], BF16)
    p.copy(eub[:, :], eu[:, :])

    if dbg == 0:
        return
    qk = p.sb("qk", [128, 4, T_], BF16)
    xin = p.sb("xin", [128, T_ + 3], F32)
    acc = p.sb("acc", [128, T_], F32)
    p.memset(xin[:, 0:3], 0.0)
    HT = T_ // 2
    for c in range(min(4, dbg)):
        p.dma([xin[:, 3 + i * 2048:3 + (i + 1) * 2048] for i in range(4)],
              [qkfm[c * 128:(c + 1) * 128, i * 2048:(i + 1) * 2048] for i in range(4)])
        for hf in range(2):
            o0 = hf * HT
            e_ = "dve"
            p.ts(acc[:, o0:o0 + HT], xin[:, 3 + o0:3 + o0 + HT], cw[:, c * 4 + 3:c * 4 + 4], None, ALU.mult, eng=e_)
            for j in range(3):
                p.stt(acc[:, o0:o0 + HT], xin[:, j + o0:j + o0 + HT], cw[:, c * 4 + j:c * 4 + j + 1],
                      acc[:, o0:o0 + HT], ALU.mult, ALU.add, eng=e_)
        if c < 2:
            p.act(qk[:, c, :], acc[:, :], AF.Silu, bias=cb[:, c:c + 1])
        else:
            p.act(acc[:, :], acc[:, :], AF.Silu, bias=cb[:, c:c + 1])
            p.ts(qk[:, c, :], acc[:, :], 128.0 ** -0.5, None, ALU.mult)

    CN = [p.sb("CN%d" % i, [128, 257], F32) for i in range(2)]
    CNs = [p.sb("CNs%d" % i, [128, 257], F32) for i in range(2)]
    CNb = [p.sb("CNb%d" % i, [128, 257], BF16) for i in range(2)]
    for i in range(2):
        p.memset(CN[i][:, :], 0.0)
        p.memset(CNb[i][:, :], 0.0)
    vraw = [p.sb("vraw%d" % i, [128, 512], BF16) for i in range(3)]
    ogt = [p.sb("ogt%d" % i, [128, 512], F32) for i in range(3)]
    vp = [p.sb("vp%d" % i, [128, 257], BF16) for i in range(4)]
    PT = [p.sb("PT%d" % i, [128, 128], BF16) for i in range(4)]
    ktm = [p.sb("ktm%d" % i, [128, 128], BF16) for i in range(4)]
    dn = [p.sb("dn%d" % i, [128, 1], F32) for i in range(4)]
    dn2 = [p.sb("dnb%d" % i, [128, 1], F32) for i in range(4)]
    hbf = [p.sb("hbf%d" % i, [128, 256], BF16) for i in range(4)]
    hTs = [p.sb("hTs%d" % i, [128, 256], BF16) for i in range(4)]
    k = 0
    for j in range(nch):
        cs = slice(j * 128, (j + 1) * 128)
        vr = vraw[j % 3]
        og = ogt[j % 3]
        p.dma(vr[:, :], vtm[cs, :])
        p.dma(og[:, :], ogtm[cs, :])
        p.act(og[:, :], og[:, :], AF.Exp, scale=-1.0)
        p.ts(og[:, :], og[:, :], 1.0, None, ALU.add)
        p.recip(og[:, :], og[:, :])
        for hd in range(2):
            col = j * 2 + hd
            b4 = k % 4
            k += 1
            qT = qk[:, hd, cs]
            kT = qk[:, 2 + hd, cs]
            p.mm(A[hd][:, 0:128], kT, qT)
            p.tt(PT[b4][:, :], A[hd][:, 0:128], tri[:, :], ALU.mult)
            p.tr(pTk[:, hd * 128:(hd + 1) * 128], kT, ident[:, :])
            p.copy(ktm[b4][:, :], pTk[:, hd * 128:(hd + 1) * 128], eng="act")
            p.act(vp[b4][:, 0:256], vr[:, hd * 256:(hd + 1) * 256], AF.Copy, scale=eu[:, col:col + 1])
            p.copy(vp[b4][:, 256:257], eub[:, col:col + 1])
            p.mm(B[hd][:, 0:257], qT, CNb[hd][:, :], start=True, stop=False)
            p.mm(B[hd][:, 0:257], PT[b4][:, :], vp[b4][:, :], start=False, stop=True)
            p.mm(Cn[hd][:, 0:257], ktm[b4][:, :], vp[b4][:, :])
            if j == 0:
                p.copy(CNs[hd][:, :], Cn[hd][:, 0:257])
            else:
                pc = col - 2
                p.stt(CNs[hd][:, :], CNs[hd][:, :], ebl[:, pc:pc + 1], Cn[hd][:, 0:257], ALU.mult, ALU.add)
            p.act(CNb[hd][:, :], CNs[hd][:, :], AF.Copy, scale=ebl[:, col:col + 1])
            d_ = dn[b4]
            p.tt(d_[:, :], B[hd][:, 256:257], eb[:, col:col + 1], ALU.mult)
            p.stt(dn2[b4][:, :], d_[:, :], -1.0, d_[:, :], ALU.mult, ALU.max)
            p.ts(d_[:, :], dn2[b4][:, :], 1.0, None, ALU.max)
            p.recip(d_[:, :], d_[:, :])
            p.tt(d_[:, :], d_[:, :], eb[:, col:col + 1], ALU.mult)
            p.stt(hbf[b4][:, :], B[hd][:, 0:256], d_[:, 0:1], og[:, hd * 256:(hd + 1) * 256], ALU.mult, ALU.mult)
            for c in range(2):
                p.tr(pTh[:, hd * 256 + c * 128:hd * 256 + (c + 1) * 128], hbf[b4][:, c * 128:(c + 1) * 128], ident[:, :])
            p.copy(hTs[b4][:, :], pTh[:, hd * 256:(hd + 1) * 256], eng="act")
            r0 = (j // 32) * 512 + hd * 256
            cc = (j % 32) * 128
            p.dma([xi[r0 + c * 128:r0 + (c + 1) * 128, cc:cc + 128] for c in range(2)],
                  [hTs[b4][:, c * 128:(c + 1) * 128] for c in range(2)])


def c_perm():
    fmc = []
    for hp in range(2):
        for base in (0, 512):
            for hd in range(2):
                h = 2 * hp + hd
                fmc += list(range(base + h * 128, base + (h + 1) * 128))
    vcols = list(range(1024, 2048))
    ogcols = list(range(2048, 3072))
    gcols = list(range(3072, 3080))
    return fmc, vcols, ogcols, gcols


def mlstm_inputs(hp, qkfm, vtm, ogtm, gtm, conv_w, conv_b, ib, fb, consts):
    d = dict(consts)
    d["qkfm"] = None if qkfm is None else np.ascontiguousarray(qkfm)
    d["vtm"] = None if vtm is None else np.ascontiguousarray(vtm)
    d["ogtm"] = None if ogtm is None else np.ascontiguousarray(ogtm)
    d["gtm"] = None if gtm is None else np.ascontiguousarray(gtm)
    fmc, _, _, _ = c_perm()
    ch = np.asarray(fmc[hp * 512:(hp + 1) * 512]).reshape(4, 128)
    cwv = np.asarray(conv_w, np.float32)[:, ch]
    d["cw"] = np.ascontiguousarray(cwv.transpose(2, 1, 0).reshape(128, 16))
    d["cb"] = np.ascontiguousarray(np.asarray(conv_b, np.float32)[ch].T)
    nchk = SEQ // 128
    d["ibf"] = np.ascontiguousarray(np.tile(np.asarray(ib[2 * hp:2 * hp + 2], np.float32)[None, :], (128, nchk)))
    d["fbf"] = np.ascontiguousarray(np.tile(np.asarray(fb[2 * hp:2 * hp + 2], np.float32)[None, :], (128, nchk)))
    return d


I32 = mybir.dt.int32
NTC = BATCH * SEQ // NCORES
MLSTM_CONST_SPECS = (("tri", [128, 128], F32), ("ones", [128, 128], F32), ("cw", [128, 16], F32),
                     ("cb", [128, 4], F32), ("ibf", [128, 128], F32), ("fbf", [128, 128], F32))
ATTN_IN_SPECS = (("gbias", [128, NB * 12], F32), ("sinks", [128, 4], F32), ("posk", [64, 32], F32),
                 ("posv", [64, 32], F32), ("w1k", [64, 2048], F32), ("w1v", [64, 2048], F32),
                 ("w2k", [64, 64], F32), ("w2v", [64, 64], F32))


def build_fused():
    p = Prog()
    ext = lambda nm, shp, dt: p.dram(nm, shp, dt, "ExternalInput")
    scr = lambda nm, shp, dt: p.dram(nm, shp, dt, "Internal")
    xb = ext("xb", [SEQ, D], F32)
    xh = ext("xh", [NTC, D], F32)
    W0 = ext("W0", [D, 1036], F32)
    W1 = ext("W1", [D, 1540], F32)
    g0, gf0, g1, gf1, gfin = (ext(n, [128, 1024], F32) for n in ("g0", "gf0", "g1", "gf1", "gfin"))
    ai = {}
    for nm, shp, dt in ATTN_CONST_SPECS + ATTN_IN_SPECS:
        ai[nm] = ext(nm, shp, dt)
    ident = ai["ident"]
    mi = {"ident": ident}
    for nm, shp, dt in MLSTM_CONST_SPECS:
        mi[nm] = ext(nm, shp, dt)
    Wo0, Wo1 = ext("Wo0", [D, D], F32), ext("Wo1", [D, D], F32)
    Wg0, Wu0, Wg1, Wu1 = (ext(n, [D, FFN], F32) for n in ("Wg0", "Wu0", "Wg1", "Wu1"))
    Wd0, Wd1 = ext("Wd0", [FFN, D], F32), ext("Wd1", [FFN, D], F32)
    out = p.dram("out", [NTC, D], F32, "ExternalOutput")
    for t in (xb, xh):
        t.track = False
    s_fm = scr("s_fm", [832, SEQ], BF16)
    s_tmv = scr("s_tmv", [SEQ, 192], BF16)
    s_gts = scr("s_gts", [SEQ, 12], F32)
    xi2 = scr("xi2", [1024, NTC], BF16)
    s_h1 = scr("s_h1", [NTC, D], F32)
    s_h2 = scr("s_h2", [NTC, D], F32)
    xi3 = scr("xi3", [1024, NTC], BF16)
    s_qk = scr("s_qk", [512, SEQ], F32)
    s_v = scr("s_v", [SEQ, 512], BF16)
    s_og = scr("s_og", [SEQ, 512], F32)
    s_g = scr("s_g", [SEQ, 4], F32)
    xi4 = scr("xi4", [1024, NTC], BF16)
    s_h3 = scr("s_h3", [NTC, D], F32)
    for t in (s_fm, s_tmv, s_gts, xi2, s_h1, s_h2, xi3, s_qk, s_v, s_og, s_g, xi4, s_h3):
        t.track = False
    G2 = scr("G2", [NCORES * 1024, NTC], BF16)
    G3 = scr("G3", [NCORES * 1024, NTC], BF16)
    G4 = scr("G4", [NCORES * 1024, NTC], BF16)
    groups = [list(range(NCORES))]
    memo = {}

    def base_pairrows(e):
        k = ("a", p.emit_id)
        if k not in memo:
            pid = e.partition_id()
            memo[k] = e.snap((pid - pid % 2) * 1024 + (pid % 2) * 512, min_val=0, max_val=6 * 1024 + 512)
        return memo[k]

    def base_pair(e):
        k = ("b", p.emit_id)
        if k not in memo:
            pid = e.partition_id()
            memo[k] = e.snap((pid - pid % 2) * 1024, min_val=0, max_val=6 * 1024)
        return memo[k]

    def gather(Gt, xi):
        p.collective("AllGather", Gt.v(Gt.h.ap().opt()), xi.v(xi.h.ap().opt()), groups)

    p.begin_phase()
    phase_proj(p, SEQ, 832, BF16, [("v", 192, BF16), ("g", 12, F32)], xb, W0, g0, ident, s_fm, [s_tmv, s_gts])
    p.end_phase()
    p.begin_phase()
    phase_attn(p, s_fm, s_tmv, s_gts, ai, xi2)
    p.end_phase()
    p.begin_phase()
    gather(G2, xi2)
    phase_outproj(p, NTC, xh, G2, base_pairrows, Wo0, s_h1)
    p.end_phase()
    p.begin_phase()
    phase_ffn(p, NTC, s_h1, Wg0, Wu0, Wd0, gf0, ident, s_h2, nxt=(g1, xi3))
    p.end_phase()
    p.begin_phase()
    gather(G3, xi3)
    phase_proj(p, SEQ, 512, F32, [("v", 512, BF16), ("og", 512, F32), ("g", 4, F32)], None, W1, None, None,
               s_qk, [s_v, s_og, s_g], pre=(G3, base_pair))
    p.end_phase()
    p.begin_phase()
    phase_mlstm(p, s_qk, s_v, s_og, s_g, mi, xi4)
    p.end_phase()
    p.begin_phase()
    gather(G4, xi4)
    phase_outproj(p, NTC, s_h2, G4, base_pairrows, Wo1, s_h3)
    p.end_phase()
    p.begin_phase()
    phase_ffn(p, NTC, s_h3, Wg1, Wu1, Wd1, gf1, ident, out, final=True, gfin_d=gfin)
    return p


def kernel(x, norm_mix, norm_ffn, ffn_w_gate, ffn_w_up, ffn_w_down, ab_w_in, ab_gate_bias, nsa_pos_k, nsa_pos_v,
           nsa_cmp_k_w1, nsa_cmp_k_w2, nsa_cmp_v_w1, nsa_cmp_v_w2, swa_sinks, ab_w_out, c_w_in, c_conv_w, c_conv_b,
           c_igate_bias, c_fgate_bias, c_w_out, final_norm):
    f32 = lambda a: np.ascontiguousarray(np.asarray(a, np.float32))
    x = f32(x)
    fmc, tmc, gc = ab_perm()
    fmc1, vcols, ogcols, gcols = c_perm()
    aconst = attn_consts()
    mconst = mlstm_consts()
    common = dict(
        g0=g_rep_of(norm_mix[0]), gf0=g_rep_of(norm_ffn[0]), g1=g_rep_of(norm_mix[1]), gf1=g_rep_of(norm_ffn[1]),
        gfin=np.ascontiguousarray(np.broadcast_to(np.asarray(final_norm, np.float32)[None, :], (128, D))),
        Wo0=f32(np.asarray(ab_w_out[0])[list(range(0, 256)) + list(range(512, 768)) + list(range(256, 512))
                                          + list(range(768, 1024))]),
        Wo1=f32(c_w_out[0]),
        Wg0=f32(ffn_w_gate[0]), Wu0=f32(ffn_w_up[0]), Wd0=f32(ffn_w_down[0]),
        Wg1=f32(ffn_w_gate[1]), Wu1=f32(ffn_w_up[1]), Wd1=f32(ffn_w_down[1]),
    )
    in_maps = []
    for c in range(NCORES):
        b, g = c // 2, c % 2
        d = dict(common)
        d["xb"] = np.ascontiguousarray(x[b])
        d["xh"] = np.ascontiguousarray(x[b, g * NTC:(g + 1) * NTC])
        cols0 = fmc[g * 832:(g + 1) * 832] + tmc[g * 192:(g + 1) * 192] + gc[g * 12:(g + 1) * 12]
        d["W0"] = f32(np.asarray(ab_w_in[0])[:, cols0])
        a_in = attn_inputs(g, None, None, None, np.asarray(ab_gate_bias[0]), np.asarray(swa_sinks[0]),
                           nsa_pos_k[0], nsa_pos_v[0], nsa_cmp_k_w1[0], nsa_cmp_k_w2[0],
                           nsa_cmp_v_w1[0], nsa_cmp_v_w2[0], aconst)
        for k in ("fm", "tmv", "gts"):
            a_in.pop(k)
        d.update(a_in)
        gsel = [gcols[2 * g], gcols[2 * g + 1], gcols[4 + 2 * g], gcols[4 + 2 * g + 1]]
        cols1 = fmc1[g * 512:(g + 1) * 512] + vcols[g * 512:(g + 1) * 512] + ogcols[g * 512:(g + 1) * 512] + gsel
        d["W1"] = f32(np.asarray(c_w_in[0])[:, cols1])
        m_in = mlstm_inputs(g, None, None, None, None, c_conv_w[0], c_conv_b[0],
                            np.asarray(c_igate_bias[0]), np.asarray(c_fgate_bias[0]), mconst)
        for k in ("qkfm", "vtm", "ogtm", "gtm", "ident"):
            m_in.pop(k)
        d.update(m_in)
        in_maps.append(d)
    p = build_fused()
    res = run_prog(p, in_maps)
    out = np.concatenate([np.asarray(res[c]["out"]) for c in range(NCORES)], axis=0)
    return out.reshape(BATCH, SEQ, D).astype(np.float32)
```

```python
import numpy as np
import ml_dtypes
from contextlib import ExitStack
import concourse.bass as bass
import concourse.mybir as mybir
from concourse.bass_utils import run_bass_kernel_spmd

F32 = mybir.dt.float32
BF16 = mybir.dt.bfloat16
AF = mybir.ActivationFunctionType
ALU = mybir.AluOpType
AX = mybir.AxisListType
NPBF = ml_dtypes.bfloat16

D = 1024
SEQ = 8192
BATCH = 4
NCORES = 8
FFN = 2816
EPS = 1e-6


class V:
    __slots__ = ("tile", "ap")

    def __init__(self, tile, ap):
        self.tile = tile
        self.ap = ap


class T:
    def __init__(self, prog, handle, name):
        self.p = prog
        self.h = handle
        self.name = name
        self.lw = None
        self.rd = []
        self.dsem = None
        self.dcnt = 0
        self.skey = None
        self.track = True
        self.onchip = True
        self.psum = False

    def __getitem__(self, idx):
        return V(self, self.h[idx])

    def v(self, ap):
        return V(self, ap)

    def all(self):
        return V(self, self.h[:])


ENG = ("pe", "act", "dve", "pool", "sp")


class Prog:
    def __init__(self):
        self.nc = bass.Bass("TRN2", target_bir_lowering=False)
        self.es = ExitStack()
        self.ops = {e: [] for e in ENG}
        self.cnt = {e: 0 for e in ENG}
        self.waited = {e: {} for e in ENG}
        self.sems = {}
        self.nsem = 0
        for e in ENG:
            self.sems[e] = self.es.enter_context(self.nc.semaphore("s_" + e))
        self.out_tokens = []
        self.uid = 0
        self.pes = None
        self.phase_tiles = []
        self.sempool = []
        self.semcount = {}
        self.barrier = {}
        self.dyn = {}
        self.emit_id = 0

    def dram(self, name, shape, dt, kind):
        if kind == "Internal":
            h = self.nc.dram_tensor(name, list(shape), dt)
        else:
            h = self.nc.dram_tensor(name, list(shape), dt, kind=kind)
        t = T(self, h, name)
        t.onchip = False
        t.track = (kind == "Internal")
        return t

    def _stack(self):
        return self.pes if self.pes is not None else self.es

    def sb(self, name, shape, dt):
        self.uid += 1
        h = self._stack().enter_context(self.nc.sbuf_tensor("%s_%d" % (name, self.uid), list(shape), dt))
        t = T(self, h, name)
        self.phase_tiles.append(t)
        return t

    def ps(self, name, shape, dt):
        self.uid += 1
        h = self._stack().enter_context(self.nc.psum_tensor("%s_%d" % (name, self.uid), list(shape), dt))
        t = T(self, h, name)
        t.psum = True
        self.phase_tiles.append(t)
        return t

    def _dsem(self, t):
        if t.dsem is None:
            if self.sempool:
                t.skey, t.dsem, t.dcnt = self.sempool.pop()
            else:
                self.nsem += 1
                t.skey = "d%d" % self.nsem
                t.dsem = self.es.enter_context(self.nc.semaphore(t.skey))
                t.dcnt = 0
                self.sems[t.skey] = t.dsem
        return t.dsem

    def begin_phase(self):
        self.pes = ExitStack()
        self.phase_tiles = []

    def end_phase(self):
        self.emit()
        for t in self.phase_tiles:
            if t.dsem is not None:
                self.sempool.append((t.skey, t.dsem, t.dcnt))
                t.dsem = None
        self.phase_tiles = []
        self.pes.close()
        self.pes = None
        bar = dict(self.semcount)
        for e in ENG:
            bar[e] = self.cnt[e]
        self.barrier = bar

    def op(self, eng, fn, writes, reads, dma=False, is_out=False, semtile=None, incamt=16):
        deps = {}

        def add(tok):
            if tok is None:
                return
            k, v = tok
            if deps.get(k, 0) < v:
                deps[k] = v

        wt = []
        for w in writes:
            if w.tile not in wt:
                wt.append(w.tile)
        rt = []
        for r in reads:
            if r.tile not in rt and r.tile not in wt:
                rt.append(r.tile)
        for t in rt:
            if t.track:
                add(t.lw)
                if t.psum:
                    for r in t.rd:
                        if r[0] != eng:
                            add(r)
        for t in wt:
            if t.track:
                add(t.lw)
                for r in t.rd:
                    add(r)
        waits = []
        for k, v in deps.items():
            if k == eng and eng == "pe" and not dma:
                continue
            if self.waited[eng].get(k, 0) >= v:
                continue
            self.waited[eng][k] = v
            waits.append((self.sems[k], v))
        if dma:
            n = dma if isinstance(dma, int) and dma is not True else 1
            sem = self._dsem(semtile)
            semtile.dcnt += incamt * n
            tok = (semtile.skey, semtile.dcnt)
            self.semcount[semtile.skey] = semtile.dcnt
            inc = (sem, incamt)
        else:
            self.cnt[eng] += 1
            tok = (eng, self.cnt[eng])
            inc = (self.sems[eng], 1)
        self.ops[eng].append((waits, fn, inc))
        for t in wt:
            if t.track:
                t.lw = tok
                t.rd = []
        for t in rt:
            if t.track:
                t.rd.append(tok)
        if is_out:
            self.out_tokens.append(tok)
        return tok

    def dma(self, out, in_, eng="sp", is_out=False):
        outs = out if isinstance(out, list) else [out]
        ins = in_ if isinstance(in_, list) else [in_]
        pairs = [(o.ap, i.ap) for o, i in zip(outs, ins)]
        semtile = outs[0].tile if outs[0].tile.onchip else ins[0].tile
        n = len(pairs)

        def fn(e):
            r = []
            for o, i in pairs:
                if callable(o):
                    o = o(e)
                if callable(i):
                    i = i(e)
                r.append(e.dma_start(out=o, in_=i))
            return r
        return self.op(eng, fn, outs, ins, dma=n, is_out=is_out, semtile=semtile)

    def collective(self, kind, out, in_, groups):
        o, i = out.ap, in_.ap

        def fn(e):
            return [e.collective_compute(kind, ALU.bypass, replica_groups=groups, ins=[i], outs=[o])]
        return self.op("pool", fn, [out], [in_], dma=1, semtile=out.tile, incamt=1)

    def mm(self, out, lhsT, rhs, start=True, stop=True, skip=False):
        o, l, r = out.ap, lhsT.ap, rhs.ap
        if skip:
            return self.op("pe", lambda e: e.matmul(o, l, r, start=start, stop=stop, skip_group_check=True),
                           [out], [lhsT, rhs])
        return self.op("pe", lambda e: e.matmul(o, l, r, start=start, stop=stop), [out], [lhsT, rhs])

    def tr(self, out, in_, ident):
        o, i, d = out.ap, in_.ap, ident.ap
        return self.op("pe", lambda e: e.transpose(o, i, d), [out], [in_, ident])

    def act(self, out, in_, func, bias=None, scale=None, accum=None, eng="act"):
        o, i = out.ap, in_.ap
        kw = {}
        rd = [in_]
        wr = [out]
        if bias is not None:
            if isinstance(bias, V):
                kw["bias"] = bias.ap
                rd.append(bias)
            else:
                kw["bias"] = bias
        if scale is not None:
            if isinstance(scale, V):
                kw["scale"] = scale.ap
                rd.append(scale)
            else:
                kw["scale"] = scale
        if accum is not None:
            kw["accum_out"] = accum.ap
            wr.append(accum)
        return self.op("act", lambda e: e.activation(o, i, func, **kw), wr, rd)

    def tt(self, out, a, b, op, eng="dve"):
        o, x, y = out.ap, a.ap, b.ap
        return self.op(eng, lambda e: e.tensor_tensor(o, x, y, op), [out], [a, b])

    def ts(self, out, a, s1, s2, op0, op1=None, eng="dve", accum=None):
        o, x = out.ap, a.ap
        rd = [a]
        wr = [out]
        if isinstance(s1, V):
            rd.append(s1)
            s1 = s1.ap
        if isinstance(s2, V):
            rd.append(s2)
            s2 = s2.ap
        kw = {}
        if op1 is not None:
            kw["op1"] = op1
        if accum is not None:
            kw["accum_out"] = accum.ap
            wr.append(accum)
        return self.op(eng, lambda e: e.tensor_scalar(o, x, s1, s2, op0, **kw), wr, rd)

    def stt(self, out, a, s, b, op0, op1, eng="dve"):
        o, x, y = out.ap, a.ap, b.ap
        rd = [a, b]
        if isinstance(s, V):
            rd.append(s)
            s = s.ap
        return self.op(eng, lambda e: e.scalar_tensor_tensor(o, x, s, y, op0, op1), [out], rd)

    def copy(self, out, in_, eng="dve"):
        o, i = out.ap, in_.ap
        if eng == "act":
            return self.op("act", lambda e: e.copy(o, i), [out], [in_])
        return self.op(eng, lambda e: e.tensor_copy(o, i), [out], [in_])

    def memset(self, out, val, eng="dve"):
        o = out.ap
        return self.op(eng, lambda e: e.memset(o, val), [out], [])

    def recip(self, out, in_):
        o, i = out.ap, in_.ap
        return self.op("dve", lambda e: e.reciprocal(o, i), [out], [in_])

    def emit(self, final=False):
        nc = self.nc
        self.emit_id += 1
        ops = self.ops
        sems = self.sems
        bar = self.barrier
        self.barrier = {}
        fin = {}
        if final:
            for k, v in self.out_tokens:
                fin[k] = max(fin.get(k, 0), v)
        engobj = {"pe": "tensor", "act": "scalar", "dve": "vector", "pool": "gpsimd", "sp": "sync"}
        waited = self.waited
        with nc.Block() as block:
            def mk(ename):
                def body(e):
                    for k, v in bar.items():
                        if v > 0 and k != ename:
                            e.wait_ge(sems[k], v)
                    for waits, fn, inc in ops[ename]:
                        for s_, v in waits:
                            e.wait_ge(s_, v)
                        r = fn(e)
                        if isinstance(r, list):
                            for x in r:
                                x.then_inc(inc[0], inc[1])
                        else:
                            r.then_inc(inc[0], inc[1])
                    if ename == "sp":
                        for k, v in fin.items():
                            e.wait_ge(sems[k], v)
                return body
            for ename in ENG:
                getattr(block, engobj[ename])(mk(ename))
        for ename in ENG:
            for k, v in bar.items():
                if waited[ename].get(k, 0) < v:
                    waited[ename][k] = v
            ops[ename] = []

    def finish(self):
        if self.pes is not None:
            self.emit(final=True)
            self.pes.close()
            self.pes = None
        else:
            self.emit(final=True)
        self.es.close()
        return self.nc


def run_prog(prog, in_maps):
    nc = prog.finish()
    res = run_bass_kernel_spmd(nc, in_maps, core_ids=list(range(len(in_maps))))
    return res.results


def load_weight_bf16(p, wdram, wb, K, N, stage, col0=0, engs=("dve", "act")):
    SW = stage[0].h.shape[1]
    i = 0
    for kc in range(K // 128):
        for c0 in range(0, N, SW):
            w = min(SW, N - c0)
            st = stage[i % len(stage)]
            p.dma(st[:, 0:w], wdram[kc * 128:(kc + 1) * 128, c0:c0 + w])
            p.copy(wb[:, kc, col0 + c0:col0 + c0 + w], st[:, 0:w], eng=engs[i % len(engs)])
            i += 1


def rms_to_fm(p, xt, hnT, tslot, g_rep, ident, scr, xs, pT, ssq, rstd):
    p.act(scr[:, :], xt[:, :], AF.Square, accum=ssq[:, 0:1])
    p.act(rstd[:, 0:1], ssq[:, 0:1], AF.Sqrt, bias=EPS, scale=1.0 / D)
    p.recip(rstd[:, 0:1], rstd[:, 0:1])
    p.ts(xs[:, :], xt[:, :], rstd[:, 0:1], None, ALU.mult)
    for c in range(8):
        p.tr(pT[:, c * 128:(c + 1) * 128], xs[:, c * 128:(c + 1) * 128], ident[:, :])
    p.tt(hnT[:, 0:8, tslot * 128:(tslot + 1) * 128],
         pT.v(pT.h[:, :].rearrange("p (c t) -> p c t", c=8)),
         g_rep.v(g_rep.h[:, :].rearrange("p (c t) -> p c t", c=8)), ALU.mult)


def phase_proj(p, NT, CF, fm_dt, tm_segs, h, W, g_rep_d, ident_d, zfm, ztm, G=512, pre=None, is_out=False):
    CT = sum(w for _, w, _ in tm_segs)
    N = CF + CT

    wb = p.sb("wb", [128, 8, N], BF16)
    stage = [p.sb("stage%d" % i, [128, 1024], F32) for i in range(4)]
    g_rep = p.sb("g_rep_sb", [128, 1024], F32)
    ident = p.sb("ident_sb", [128, 128], BF16)
    xt = [p.sb("xt%d" % i, [128, D], F32) for i in range(2)]
    scr = p.sb("scr", [128, D], BF16)
    xs = [p.sb("xs%d" % i, [128, D], BF16) for i in range(2)]
    ssq = [p.sb("ssq%d" % i, [128, 1], F32) for i in range(2)]
    rstd = [p.sb("rstd%d" % i, [128, 1], F32) for i in range(2)]
    if pre is None:
        hnT = [p.sb("hnT%d" % i, [128, 8, G], BF16) for i in range(2)]
    else:
        hnT = [None, None]
        hh = p.sb("hh", [128, 8, 4096], BF16)
    ofm = [p.sb("ofm%d" % i, [128, G], fm_dt) for i in range(3)]
    otm = [[p.sb("otm_%s%d" % (nm, i), [128, w], dt) for i in range(2)] for nm, w, dt in tm_segs]
    pT = [p.ps("pT%d" % i, [128, 1024], BF16) for i in range(2)]
    pm = [p.ps("pm%d" % i, [128, 512], F32) for i in range(4)]

    if pre is None:
        p.dma(g_rep[:, :], g_rep_d[:, :])
        p.dma(ident[:, :], ident_d[:, :])
    load_weight_bf16(p, W, wb, D, N, stage)

    TPG = G // 128
    st_it = [0]
    k_pm = 0
    k_of = 0
    NG = NT // G

    def load_tile(gi, ti):
        t0 = gi * G + ti * 128
        p.dma(xt[ti % 2][:, :], h[t0:t0 + 128, :])

    def rms_tile(gi, ti):
        b_ = ti % 2
        rms_to_fm(p, xt[b_], hnT[gi % 2], ti, g_rep, ident, scr, xs[b_], pT[b_], ssq[b_], rstd[b_])

    def early_loads(gi):
        for ti in range(min(2, TPG)):
            load_tile(gi, ti)

    def rms_group(gi):
        for ti in range(TPG):
            rms_tile(gi, ti)
            if ti + 2 < TPG:
                load_tile(gi, ti + 2)

    if pre is None:
        early_loads(0)
        rms_group(0)
    for gi in range(NG):
        hb = hnT[gi % 2]
        if pre is None and gi + 1 < NG:
            early_loads(gi + 1)
        if pre is not None:
            G3, basefn = pre
            half, c0 = (gi * G) // 4096, (gi * G) % 4096
            if c0 == 0:
                p.dma(hh[:, :, :],
                      G3.v((lambda e, half=half: G3.h[bass.ds(basefn(e) + half * 1024, 1024), :]
                            .rearrange("(c p) n -> p c n", p=128))))
            hs = lambda c, a, b_, c0=c0: hh[:, c, c0 + a:c0 + b_]
        else:
            hs = lambda c, a, b_, hb=hb: hb[:, c, a:b_]
        nfm = len(range(0, CF, 128))
        for mi, m0 in enumerate(range(0, CF, 128)):
            if pre is None and gi + 1 < NG and mi == nfm // 2:
                rms_group(gi + 1)
            mw = min(128, CF - m0)
            ps_ = pm[k_pm % 4]
            k_pm += 1
            for c in range(8):
                p.mm(ps_[0:mw, 0:G], wb[:, c, m0:m0 + mw], hs(c, 0, G), start=(c == 0), stop=(c == 7))
            o = ofm[k_of % 3]
            if k_of % 2 == 0:
                p.copy(o[0:mw, :], ps_[0:mw, 0:G], eng="act")
            else:
                p.copy(o[0:mw, :], ps_[0:mw, 0:G], eng="dve")
            k_of += 1
            p.dma(zfm[m0:m0 + mw, gi * G:(gi + 1) * G], o[0:mw, :], is_out=is_out)
        for ti in range(TPG):
            t0 = gi * G + ti * 128
            off = CF
            for si, (nm, w, dt) in enumerate(tm_segs):
                o = otm[si][ti % 2]
                for c0_ in range(0, w, 512):
                    cw = min(512, w - c0_)
                    ps_ = pm[k_pm % 4]
                    k_pm += 1
                    for c in range(8):
                        p.mm(ps_[:, 0:cw], hs(c, ti * 128, (ti + 1) * 128),
                             wb[:, c, off + c0_:off + c0_ + cw], start=(c == 0), stop=(c == 7))
                    if k_of % 2 == 0:
                        p.copy(o[:, c0_:c0_ + cw], ps_[:, 0:cw], eng="act")
                    else:
                        p.copy(o[:, c0_:c0_ + cw], ps_[:, 0:cw], eng="dve")
                    k_of += 1
                p.dma(ztm[si][t0:t0 + 128, :], o[:, :], is_out=is_out)
                off += w


def phase_outproj(p, NT, x, Gt, basefn, Wo, h1, G=512):
    wb = p.sb("wb", [128, 8, D], BF16)
    stage = [p.sb("stage%d" % i, [128, 1024], F32) for i in range(4)]
    ob = p.sb("ot", [128, 8, NT], BF16)
    xt = [p.sb("xt%d" % i, [128, D], F32) for i in range(3)]
    pm = [p.ps("pm%d" % i, [128, 512], F32) for i in range(4)]
    p.dma([ob[:, r * 4:(r + 1) * 4, :] for r in range(2)],
          [Gt.v((lambda e, r=r: Gt.h[bass.ds(basefn(e) + r * 1024, 512), :].rearrange("(c p) n -> p c n", p=128)))
           for r in range(2)])
    load_weight_bf16(p, Wo, wb, D, D, stage)
    k = 0
    it = 0
    for ti in range(NT // 128):
        t0 = ti * 128
        xb = xt[it % 3]
        it += 1
        p.dma(xb[:, :], x[t0:t0 + 128, :])
        for half in range(2):
            ps_ = pm[k % 4]
            k += 1
            for c in range(8):
                p.mm(ps_[:, :], ob[:, c, t0:t0 + 128], wb[:, c, half * 512:(half + 1) * 512],
                     start=(c == 0), stop=(c == 7))
            p.tt(xb[:, half * 512:(half + 1) * 512], ps_[:, :], xb[:, half * 512:(half + 1) * 512], ALU.add)
        p.dma(h1[t0:t0 + 128, :], xb[:, :], is_out=False)


def phase_ffn(p, NT, h1, Wg, Wu, Wd, g_rep_d, ident_d, h2, final=False, gfin_d=None, nxt=None, G=256):
    NF = FFN // 128
    wg = p.sb("wg", [128, 8, FFN], BF16)
    wu = p.sb("wu", [128, 8, FFN], BF16)
    wd = p.sb("wd", [128, NF, D], BF16)
    stage = [p.sb("stage%d" % i, [128, 1024], F32) for i in range(4)]
    g_rep = p.sb("g_rep_sb", [128, 1024], F32)
    ident = p.sb("ident_sb", [128, 128], BF16)
    TPG = G // 128
    xt = [p.sb("xt%d" % i, [128, D], F32) for i in range(2 * TPG)]
    scr = p.sb("scr", [128, D], BF16)
    xs = [p.sb("xs%d" % i, [128, D], BF16) for i in range(2)]
    ssq = [p.sb("ssq%d" % i, [128, 1], F32) for i in range(2)]
    rstd = [p.sb("rstd%d" % i, [128, 1], F32) for i in range(2)]
    hnT = [p.sb("hnT%d" % i, [128, 8, G], BF16) for i in range(2)]
    aT = p.sb("aT", [128, NF, G], BF16)
    sil = [p.sb("sil%d" % i, [128, G], F32) for i in range(2)]
    if final:
        gfin = p.sb("gfin_sb", [128, D], F32)
        ssq2 = p.sb("ssq2", [128, 1], F32)
        rstd2 = p.sb("rstd2", [128, 1], F32)
    if nxt is not None:
        g2_rep = p.sb("g2_rep_sb", [128, 1024], F32)
        p.dma(g2_rep[:, :], nxt[0][:, :])
        hn2 = [p.sb("hn2_%d" % i, [128, 8, 128], BF16) for i in range(2)]
    pT = [p.ps("pT%d" % i, [128, 1024], BF16) for i in range(2)]
    pg = [p.ps("pg%d" % i, [128, 512], F32) for i in range(2)]
    pu = [p.ps("pu%d" % i, [128, 512], F32) for i in range(2)]
    pd = [p.ps("pd%d" % i, [128, 512], F32) for i in range(2)]

    p.dma(g_rep[:, :], g_rep_d[:, :])
    p.dma(ident[:, :], ident_d[:, :])
    if final:
        p.dma(gfin[:, :], gfin_d[:, :])
    load_weight_bf16(p, Wg, wg, D, FFN, stage)
    load_weight_bf16(p, Wu, wu, D, FFN, stage)
    load_weight_bf16(p, Wd, wd, FFN, D, stage)

    it = 0
    kf = 0
    kd = 0
    NG = NT // G

    def load_group(gi):
        xs_ = []
        for ti in range(TPG):
            t0 = gi * G + ti * 128
            xb = xt[(gi * TPG + ti) % (2 * TPG)]
            p.dma(xb[:, :], h1[t0:t0 + 128, :])
            xs_.append(xb)
        return xs_

    def rms_group(gi, xs_):
        for ti, xb in enumerate(xs_):
            b_ = (gi * TPG + ti) % 2
            rms_to_fm(p, xb, hnT[gi % 2], ti, g_rep, ident, scr, xs[b_], pT[b_], ssq[b_], rstd[b_])

    xts_next = load_group(0)
    rms_group(0, xts_next)
    for gi in range(NG):
        hb = hnT[gi % 2]
        xts = xts_next
        if gi + 1 < NG:
            xts_next = load_group(gi + 1)
        for f in range(NF):
            if gi + 1 < NG and f == NF // 2:
                rms_group(gi + 1, xts_next)
            b = kf % 2
            kf += 1
            for c in range(8):
                p.mm(pg[b][:, 0:G], wg[:, c, f * 128:(f + 1) * 128], hb[:, c, :], start=(c == 0), stop=(c == 7))
            for c in range(8):
                p.mm(pu[b][:, 0:G], wu[:, c, f * 128:(f + 1) * 128], hb[:, c, :], start=(c == 0), stop=(c == 7))
            p.act(sil[b][:, :], pg[b][:, 0:G], AF.Silu)
            p.tt(aT[:, f, :], sil[b][:, :], pu[b][:, 0:G], ALU.mult)
        for ti in range(TPG):
            t0 = gi * G + ti * 128
            xb = xts[ti]
            for half in range(2):
                ps_ = pd[kd % 2]
                kd += 1
                for f in range(NF):
                    p.mm(ps_[:, :], aT[:, f, ti * 128:(ti + 1) * 128], wd[:, f, half * 512:(half + 1) * 512],
                         start=(f == 0), stop=(f == NF - 1))
                p.tt(xb[:, half * 512:(half + 1) * 512], ps_[:, :], xb[:, half * 512:(half + 1) * 512], ALU.add)
            if final:
                p.act(scr[:, :], xb[:, :], AF.Square, accum=ssq2[:, 0:1])
                p.act(rstd2[:, 0:1], ssq2[:, 0:1], AF.Sqrt, bias=EPS, scale=1.0 / D)
                p.recip(rstd2[:, 0:1], rstd2[:, 0:1])
                p.stt(xb[:, :], xb[:, :], rstd2[:, 0:1], gfin[:, :], ALU.mult, ALU.mult)
            p.dma(h2[t0:t0 + 128, :], xb[:, :], is_out=final)
            if nxt is not None:
                b = it % 2
                it += 1
                rms_to_fm(p, xb, hn2[b], 0, g2_rep, ident, scr, xs[b], pT[b], ssq[b], rstd[b])
                p.dma([nxt[1][c * 128:(c + 1) * 128, t0:t0 + 128] for c in range(8)],
                      [hn2[b][:, c, :] for c in range(8)])


NEG = -30000.0
import os
HAMTEST = int(os.environ.get('HAMTEST', '0'))
NB = SEQ // 128
FM_QA, FM_KC, FM_VC, FM_KS, FM_KW, FM_QB, FM_KB = 0, 256, 320, 384, 448, 512, 768


def attn_consts():
    k = np.arange(128)[:, None]
    q = (np.arange(512) % 128)[None, :]
    c = {}
    c["ident"] = np.eye(128).astype(NPBF)
    c["mdiag"] = np.where(k <= q, 0.0, NEG).astype(NPBF)
    c["mfar"] = np.where(k > q, 0.0, NEG).astype(NPBF)
    cm = np.zeros((128, 17, 512), np.float32)
    for dl in range(17):
        cm[:, dl, :] = np.where(16 * k + 31 - q <= 128 * dl, 0.0, NEG)
    c["cmpmask"] = cm.reshape(128, 17 * 512).astype(NPBF)
    s = np.arange(64)[:, None]
    kk = np.arange(SEQ)[None, :]
    c["E2"] = ((kk // 64) % 64 == s).astype(np.float32).astype(NPBF)
    cs = np.arange(512)[:, None] * 16
    ss = np.arange(128)[None, :] * 64
    sm = ((cs < ss + 64) & (cs + 32 > ss)).astype(np.float32)
    sm[511, :] = 0.0
    c["selmap"] = sm.reshape(4, 128, 128).transpose(1, 0, 2).reshape(128, 512).astype(NPBF)
    ql = np.arange(128)[:, None]
    sp = np.arange(256)[None, :] - 126
    cur = ql // 64
    c["tb1"] = (sp < cur - 1).astype(np.float32)
    c["tb2"] = np.where(sp > cur, -1e30, np.where(sp >= cur - 1, 1e9, 0.0)).astype(np.float32)
    c["tb3"] = (sp <= cur).astype(np.float32)
    cv = np.ones((128, 4), np.float32)
    cv[127, 3] = 0.0
    c["cvalid"] = cv
    return c


ATTN_CONST_SPECS = (("ident", [128, 128], BF16), ("mdiag", [128, 512], BF16), ("mfar", [128, 512], BF16),
                    ("cmpmask", [128, 17 * 512], BF16), ("E2", [64, SEQ], BF16), ("selmap", [128, 512], BF16),
                    ("tb1", [128, 256], F32), ("tb2", [128, 256], F32), ("tb3", [128, 256], F32),
                    ("cvalid", [128, 4], F32))


def phase_attn(p, fm, tmv, gts, ai, xi, nblk=NB):
    T_ = SEQ
    gbias_d, sinks_d, posk_d, posv_d = ai["gbias"], ai["sinks"], ai["posk"], ai["posv"]
    w1k_d, w1v_d, w2k_d, w2v_d = ai["w1k"], ai["w1v"], ai["w2k"], ai["w2v"]
    cd = {}
    for nm, shp, dt in ATTN_CONST_SPECS:
        if nm == "E2":
            continue
        d_ = ai[nm]
        s_ = p.sb(nm + "_sb", shp, dt)
        p.dma(s_[:, :], d_[:, :])
        cd[nm] = s_
    ident, mdiag, mfar, cmpmask, selmap = (cd[k] for k in ("ident", "mdiag", "mfar", "cmpmask", "selmap"))
    tb1, tb2, tb3, cvalid = cd["tb1"], cd["tb2"], cd["tb3"], cd["cvalid"]

    ksT = p.sb("ksT", [128, T_], BF16)
    p.dma([ksT[64:128, i * 2048:(i + 1) * 2048] for i in range(4)],
          [ai["E2"][:, i * 2048:(i + 1) * 2048] for i in range(4)])
    kwT = p.sb("kwT", [128, T_], BF16)
    kbT = p.sb("kbT", [128, T_], BF16)
    p.memset(kwT[64:128, :], 0.0, eng="pool")
    p.memset(kbT[64:128, :], 0.0, eng="pool")
    kcin = p.sb("kcin", [64, T_], BF16)
    vcin = p.sb("vcin", [64, T_], BF16)
    for dst, r0 in ((ksT, FM_KS), (kwT, FM_KW), (kbT, FM_KB), (kcin, FM_KC), (vcin, FM_VC)):
        p.dma([dst[0:64, i * 2048:(i + 1) * 2048] for i in range(4)],
              [fm[r0:r0 + 64, i * 2048:(i + 1) * 2048] for i in range(4)])
    vs1 = p.sb("vs1", [128, NB, 65], BF16)
    vw1 = p.sb("vw1", [128, NB, 65], BF16)
    vb1 = p.sb("vb1", [128, NB, 65], BF16)
    for i, vt in enumerate((vs1, vw1, vb1)):
        p.memset(vt[:, :, :], 1.0, eng="pool")
        src = tmv.h[:, i * 64:(i + 1) * 64].rearrange("(j p) d -> p j d", p=128)
        p.dma([vt[:, j * 8:(j + 1) * 8, 0:64] for j in range(8)],
              [tmv.v(src[:, j * 8:(j + 1) * 8, :]) for j in range(8)])
    gate = p.sb("gate", [128, NB * 12], F32)
    gb = p.sb("gb", [128, NB * 12], F32)
    p.dma(gate.v(gate.h[:, :].rearrange("p (j c) -> p j c", c=12)),
          gts.v(gts.h[:, :].rearrange("(j p) c -> p j c", p=128)))
    p.dma(gb[:, :], gbias_d[:, :])
    p.tt(gate[:, :], gate[:, :], gb[:, :], ALU.add)
    p.act(gate[:, :], gate[:, :], AF.Exp, scale=-1.0)
    p.ts(gate[:, :], gate[:, :], 1.0, None, ALU.add)
    p.recip(gate[:, :], gate[:, :])
    gate3 = gate.h[:, :].rearrange("p (j h c) -> p j h c", h=4, c=3)
    esink = p.sb("esink", [128, 4], F32)
    p.dma(esink[:, :], sinks_d[:, :])
    p.act(esink[:, :], esink[:, :], AF.Exp)

    S = [p.ps("S%d" % i, [128, 512], F32) for i in range(3)]
    OcT = p.ps("OcT", [128, 512], F32)
    U = p.ps("U", [128, 512], F32)
    OsT = p.ps("OsT", [128, 512], F32)
    OwT = p.ps("OwbT", [128, 512], F32)
    ObT = OwT
    X = p.ps("X", [128, 512], F32)
    Xb = X.h.bitcast(BF16)
    identf = p.sb("identf", [128, 128], F32)
    p.copy(identf[:, :], ident[:, :])
    otf = [p.sb("otf%d" % i, [65, 512], F32) for i in range(4)]
    st_of = [0]

    kcT = p.sb("kcT", [128, 512], BF16)
    vc1 = p.sb("vc1", [128, 4, 65], BF16)
    stg = p.sb("stg", [64, 2048], F32)
    w1b = p.sb("w1b", [64, 2048], BF16)
    w2s = p.sb("w2s", [64, 64], F32)
    w2b = p.sb("w2b", [64, 64], BF16)
    poss = p.sb("poss", [64, 32], F32)
    posb = p.sb("posb", [64, 32], BF16)
    bcol = p.sb("bcol", [64, 1], F32)
    h1T = p.sb("h1T", [64, 512], BF16)
    p.memset(h1T[:, :], 0.0)
    p.memset(kcT[:, :], 0.0)
    p.memset(vc1[:, :, :], 0.0)
    for which, (w1d, w2d, posd, src) in enumerate(((w1k_d, w2k_d, posk_d, kcin), (w1v_d, w2v_d, posv_d, vcin))):
        p.dma(stg[:, :], w1d[:, :])
        p.copy(w1b[:, :], stg[:, :])
        p.dma(w2s[:, :], w2d[:, :])
        p.copy(w2b[:, :], w2s[:, :])
        p.dma(poss[:, :], posd[:, :])
        p.copy(posb[:, :], poss[:, :])
        for l in range(32):
            p.mm(S[0][0:64, 0:1], w1b[:, l * 64:(l + 1) * 64], posb[:, l:l + 1], start=(l == 0), stop=(l == 31))
        p.copy(bcol[:, :], S[0][0:64, 0:1])
        sv = src.h[:, :].rearrange("p (i r) -> p i r", r=16)
        for l in range(32):
            rhs = sv[:, 0:511, l] if l < 16 else sv[:, 1:512, l - 16]
            p.mm(S[1][0:64, 0:511], w1b[:, l * 64:(l + 1) * 64], src.v(rhs), start=(l == 0), stop=(l == 31))
        p.act(h1T[:, 0:511], S[1][0:64, 0:511], AF.Silu, bias=bcol[:, 0:1])
        if which == 0:
            p.mm(S[0][0:64, 0:511], w2b[:, :], h1T[:, 0:511])
            p.copy(kcT[0:64, 0:511], S[0][0:64, 0:511])
        else:
            for m in range(4):
                p.mm(S[0][:, m * 64:(m + 1) * 64], h1T[:, m * 128:(m + 1) * 128], w2b[:, :])
            p.copy(vc1[:, :, 0:64], S[0].v(S[0].h[:, 0:256].rearrange("p (m d) -> p m d", m=4)))
            p.copy(vc1[:, :, 64], cvalid[:, :])

    qa = [p.sb("qa%d" % i, [128, 512], BF16) for i in range(3)]
    qa1 = [p.sb("qa1_%d" % i, [128, 512], BF16) for i in range(3)]
    nmsw = p.sb("nmsw", [128, 128], BF16)
    for t_ in qa + qa1:
        p.memset(t_[64:128, :], 0.0)
    qb = [p.sb("qb%d" % i, [128, 512], BF16) for i in range(2)]
    for t_ in qb:
        p.memset(t_[64:128, :], 0.0)
    Pb = [p.sb("P%d" % i, [128, 512], BF16) for i in range(4)]
    nmT = [p.sb("nmT%d" % i, [128, 512], BF16) for i in range(2)]
    oacc = [p.sb("oacc%d" % i, [128, 512], F32) for i in range(2)]
    obf = p.sb("obf", [128, 512], BF16)
    oTs = [p.sb("oTs%d" % i, [128, 512], BF16) for i in range(2)]
    imp = p.sb("imp", [128, 128], F32)
    score = p.sb("score", [128, 128], F32)
    sc2 = p.sb("sc2", [128, 128], F32)
    m8 = p.sb("m8", [128, 8], F32)
    thr = p.sb("thr", [128, 1], F32)
    sel = p.sb("sel", [128, 128], F32)
    nmb = p.sb("nmb", [128, 128], BF16)
    lt = [p.sb("lt%d" % i, [128, 4], F32) for i in range(4)]
    wg_ = [p.sb("wgt%d" % i, [128, 4], F32) for i in range(4)]
    st = {"S": 0, "P": 0}

    def load_q(n):
        cs = slice(n * 128, (n + 1) * 128)
        for a in ([qa[n % 3]] if n < 32 else [qa[n % 3], qa1[n % 3]]):
            p.dma(a.v(a.h[0:64, :].rearrange("d (h t) -> d h t", h=4)),
                  fm.v(fm.h[FM_QA:FM_QA + 256, cs].rearrange("(h d) t -> d h t", d=64)))
        b = qb[n % 2]
        p.dma(b.v(b.h[0:64, :].rearrange("d (h t) -> d h t", h=4)),
              fm.v(fm.h[FM_QB:FM_QB + 256, cs].rearrange("(h d) t -> d h t", d=64)))

    pend = []

    def defer(k, fn):
        pend.append([k, fn])

    def tick():
        for e_ in pend:
            e_[0] -= 1
        while pend and pend[0][0] <= 0:
            pend.pop(0)[1]()

    def flush_pending():
        while pend:
            pend.pop(0)[1]()

    def branch(specs, OT, extra=None):
        nt = len(specs)
        banks = {}

        def emitS(i):
            ps_ = S[st["S"] % 3]
            st["S"] += 1
            banks[i] = ps_
            mms = specs[i][0]
            for idx, (l, r) in enumerate(mms):
                p.mm(ps_[:, :], l, r, start=(idx == 0), stop=(idx == len(mms) - 1))
        emitS(0)
        if nt > 1:
            emitS(1)
        tick()
        for i in range(nt):
            if i + 2 < nt:
                emitS(i + 2)
            P_ = Pb[st["P"] % 4]
            st["P"] += 1
            p.act(P_[:, :], banks[i][:, :], AF.Exp, scale=0.125)
            p.mm(OT[0:65, :], specs[i][1], P_[:, :], start=(i == 0), stop=(i == nt - 1))
            if extra is not None:
                extra(P_, i, i == 0, i == nt - 1)
            tick()

    def to_token_major(OT, then, k2=4):
        if sum(1 for e_ in pend if e_[1].__name__ == "later") >= 3:
            flush_pending()
        of = otf[st_of[0] % 4]
        st_of[0] += 1
        p.copy(of[:, :], OT[0:65, :], eng="dve")

        def later():
            for h in range(4):
                p.tr(X[:, 128 + h * 65:128 + (h + 1) * 65], of[:, h * 128:(h + 1) * 128], identf[0:65, 0:65])
            r = then()
            if r is not None:
                pend.insert(0, [k2, r])
        defer(1, later)

    class OV:
        tile = X
        h = None

        def __getitem__(self, idx):
            rs, cs = idx
            return X[rs, slice(128 + cs.start, 128 + cs.stop)]
    Otm = OV()

    def ovw(O):
        return X.v(X.h[:, 128:388].rearrange("p (h e) -> p h e", e=65))

    def norm_weights(O, k, gate_j, n):
        if gate_j is None:
            p.tt(lt[k][:, :], X.v(ovw(O).ap[:, :, 64]), esink[:, :], ALU.add)
        else:
            p.ts(lt[k][:, :], X.v(ovw(O).ap[:, :, 64]), 1e-30, None, ALU.max)
        p.recip(lt[k][:, :], lt[k][:, :])
        if gate_j is None:
            return lt[k]
        p.tt(wg_[k][:, :], lt[k][:, :], gate.v(gate3[:, n, :, gate_j]), ALU.mult)
        return wg_[k]

    def cmp_and_topk(n):
        a = qa[n % 3]
        ntc = min(4, n // 16 + 1)
        specs = []
        for m in range(ntc):
            mms = [(kcT[:, m * 128:(m + 1) * 128], a[:, :])]
            dl = n - 16 * m
            if dl <= 16:
                mms.append((ident[:, :], cmpmask[:, dl * 512:(dl + 1) * 512]))
            specs.append((mms, vc1[:, m, :]))

        def extra(P_, i, first, last):
            for h in range(4):
                p.mm(U[:, h * 128:(h + 1) * 128], P_[:, h * 128:(h + 1) * 128], selmap[:, i * 128:(i + 1) * 128],
                     start=(first and h == 0), stop=last, skip=True)
        branch(specs, OcT, extra)

        def cont():
            w = norm_weights(Otm, 0, 0, n)
            oa = oacc[n % 2]
            for h in range(4):
                p.ts(oa[:, h * 64:(h + 1) * 64], Otm[:, h * 65:h * 65 + 64], w[:, h:h + 1], None, ALU.mult)
            rl = lt[0]
            p.ts(imp[:, :], U[:, 0:128], rl[:, 0:1], None, ALU.mult)
            for h in range(1, 4):
                p.stt(imp[:, :], U[:, h * 128:(h + 1) * 128], rl[:, h:h + 1], imp[:, :], ALU.mult, ALU.add)
            u0 = 126 - 2 * n
            p.tt(score[:, :], imp[:, :], tb1[:, u0:u0 + 128], ALU.mult)
            p.tt(score[:, :], score[:, :], tb2[:, u0:u0 + 128], ALU.add)
            p.memset(score[:, 0:1], 1e9)
            so, s2o, m8o = score.h[:, :], sc2.h[:, :], m8.h[:, :]
            p.op("dve", lambda e: e.max(out=m8o, in_=so), [m8.all()], [score.all()])
            p.op("dve", lambda e: e.match_replace(out=s2o, in_to_replace=m8o, in_values=so, imm_value=-3e38),
                 [sc2.all()], [m8.all(), score.all()])
            p.op("dve", lambda e: e.max(out=m8o, in_=s2o), [m8.all()], [sc2.all()])
            tho = thr.h[:, :]
            p.op("dve", lambda e: e.tensor_reduce(tho, m8o, AX.X, ALU.min), [thr.all()], [m8.all()])
            p.stt(sel[:, :], score[:, :], thr[:, 0:1], tb3[:, u0:u0 + 128], ALU.is_ge, ALU.mult)
            p.ts(nmsw[:, 64:128], sel[:, 0:64], 1.0, -NEG, ALU.subtract, ALU.mult)
            p.ts(nmsw[:, 0:64], sel[:, 64:128], 1.0, -NEG, ALU.subtract, ALU.mult)
            if n >= 32:
                p.ts(nmb[:, :], sel[:, :], 1.0, -NEG, ALU.subtract, ALU.mult)
            return stage2

        def stage2():
            p.tr(X.v(Xb[:, 0:128]), nmsw[:, :], ident[:, :])
            for h in range(4):
                p.copy(a[64:128, h * 128:(h + 1) * 128], X.v(Xb[64:128, 0:128]), eng="dve")
            if n >= 32:
                p.tr(X.v(Xb[:, 128:256]), nmb[:, :], ident[:, :])
                a1 = qa1[n % 3]
                for h in range(4):
                    p.copy(a1[64:128, h * 128:(h + 1) * 128], X.v(Xb[64:128, 128:256]), eng="dve")
        to_token_major(OcT, cont, k2=14)

    def rest(n):
        a = qa[n % 3]
        a1 = qa1[n % 3]
        b = qb[n % 2]
        oa = oacc[n % 2]
        ks_ = lambda j: slice(j * 128, (j + 1) * 128)
        specs = []
        for j in range(max(0, n - 4), n + 1):
            mms = [(kwT[:, ks_(j)], a[:, :])]
            if j == n:
                mms.append((ident[:, :], mdiag[:, :]))
            if j == n - 4:
                mms.append((ident[:, :], mfar[:, :]))
            specs.append((mms, vw1[:, j, :]))
        branch(specs, OwT)

        def cont_w():
            w2_ = norm_weights(Otm, 2, 2, n)
            for h in range(4):
                p.stt(oa[:, h * 64:(h + 1) * 64], Otm[:, h * 65:h * 65 + 64], w2_[:, h:h + 1],
                      oa[:, h * 64:(h + 1) * 64], ALU.mult, ALU.add)
        to_token_major(OwT, cont_w)
        if any(e_[1].__name__ in ("later", "stage2") for e_ in pend):
            flush_pending()
        specs = []
        for j in range(n + 1):
            mms = [(ksT[:, ks_(j)], (a if j < 32 else a1)[:, :])]
            if j == n:
                mms.append((ident[:, :], mdiag[:, :]))
            specs.append((mms, vs1[:, j, :]))
        branch(specs, OsT)

        def cont_s():
            w1_ = norm_weights(Otm, 1, 1, n)
            for h in range(4):
                p.stt(obf[:, h * 64:(h + 1) * 64], Otm[:, h * 65:h * 65 + 64], w1_[:, h:h + 1],
                      oa[:, h * 64:(h + 1) * 64], ALU.mult, ALU.add)
        to_token_major(OsT, cont_s)
        specs = []
        for j in range(max(0, n - 1), n + 1):
            mms = [(kbT[:, ks_(j)], b[:, :])]
            if j == n:
                mms.append((ident[:, :], mdiag[:, :]))
            if j == n - 1:
                mms.append((ident[:, :], mfar[:, :]))
            specs.append((mms, vb1[:, j, :]))
        branch(specs, ObT)

        def cont_b():
            w3_ = norm_weights(Otm, 3, None, n)
            for h in range(4):
                p.ts(obf[:, 256 + h * 64:256 + (h + 1) * 64], Otm[:, h * 65:h * 65 + 64], w3_[:, h:h + 1], None, ALU.mult)
            return stage_out

        def stage_out():
            for c in range(4):
                p.tr(X.v(Xb[:, 256 + c * 128:256 + (c + 1) * 128]), obf[:, c * 128:(c + 1) * 128], ident[:, :])
            ot = oTs[n % 2]
            p.copy(ot[:, :], X.v(Xb[:, 256:768]), eng="dve")
            r0 = (n // 32) * 512
            cc = (n % 32) * 128
            p.dma([xi[r0 + c * 128:r0 + (c + 1) * 128, cc:cc + 128] for c in range(4)],
                  [ot[:, c * 128:(c + 1) * 128] for c in range(4)])
        to_token_major(ObT, cont_b, k2=5)

    load_q(0)
    cmp_and_topk(0)
    for n in range(nblk):
        if n + 1 < nblk:
            load_q(n + 1)
            cmp_and_topk(n + 1)
        rest(n)
    flush_pending()


def ab_perm():
    off = dict(qa=0, kc=512, vc=640, ks=768, vs=896, kw=1024, vw=1152, g=1280, qb=1304, kb=1816, vb=1944)
    fmc = []
    for g in range(2):
        fmc += list(range(off["qa"] + g * 256, off["qa"] + (g + 1) * 256))
        for nm in ("kc", "vc", "ks", "kw"):
            fmc += list(range(off[nm] + g * 64, off[nm] + (g + 1) * 64))
        fmc += list(range(off["qb"] + g * 256, off["qb"] + (g + 1) * 256))
        fmc += list(range(off["kb"] + g * 64, off["kb"] + (g + 1) * 64))
    tmc = []
    for g in range(2):
        for nm in ("vs", "vw", "vb"):
            tmc += list(range(off[nm] + g * 64, off[nm] + (g + 1) * 64))
    gc = list(range(1280, 1304))
    return fmc, tmc, gc


def g_rep_of(g):
    return np.ascontiguousarray(
        np.repeat(np.asarray(g, np.float32).reshape(8, 128).T[:, :, None], 128, axis=2).reshape(128, 1024))


def attn_inputs(g, fm, tmv, gts, gate_bias, sinks, pos_k, pos_v, w1k, w2k, w1v, w2v, consts):
    d = dict(consts)
    d["fm"] = None if fm is None else np.ascontiguousarray(fm)
    d["tmv"] = None if tmv is None else np.ascontiguousarray(tmv)
    d["gts"] = None if gts is None else np.ascontiguousarray(gts)
    d["gbias"] = np.ascontiguousarray(np.tile(np.asarray(gate_bias[g * 12:(g + 1) * 12], np.float32)[None, :], (128, NB)))
    d["sinks"] = np.ascontiguousarray(np.broadcast_to(np.asarray(sinks[g * 4:(g + 1) * 4], np.float32)[None, :], (128, 4)))
    d["posk"] = np.ascontiguousarray(np.asarray(pos_k, np.float32).T)
    d["posv"] = np.ascontiguousarray(np.asarray(pos_v, np.float32).T)
    r1 = lambda w: np.ascontiguousarray(np.asarray(w, np.float32).reshape(32, 64, 64).transpose(1, 0, 2).reshape(64, 2048))
    d["w1k"] = r1(w1k)
    d["w1v"] = r1(w1v)
    d["w2k"] = np.ascontiguousarray(np.asarray(w2k, np.float32))
    d["w2v"] = np.ascontiguousarray(np.asarray(w2v, np.float32))
    return d


def mlstm_consts():
    s = np.arange(128)[:, None]
    t = np.arange(128)[None, :]
    c = {}
    c["ident"] = np.eye(128).astype(NPBF)
    c["tri"] = (s <= t).astype(np.float32)
    c["ones"] = np.ones((128, 128), np.float32)
    return c


def phase_mlstm(p, qkfm, vtm, ogtm, gtm, mi, xi, nch=SEQ // 128, dbg=99):
    T_ = SEQ
    NCH = SEQ // 128
    cd = {}
    for nm, shp, dt in (("ident", [128, 128], BF16), ("tri", [128, 128], F32), ("ones", [128, 128], F32),
                        ("cw", [128, 16], F32), ("cb", [128, 4], F32), ("ibf", [128, NCH * 2], F32),
                        ("fbf", [128, NCH * 2], F32)):
        if True:
            d_ = mi[nm]
        s_ = p.sb(nm + "_sb", shp, dt)
        p.dma(s_[:, :], d_[:, :])
        cd[nm] = s_
    ident, tri, ones, cw, cb, ibf, fbf = (cd[k] for k in ("ident", "tri", "ones", "cw", "cb", "ibf", "fbf"))

    A = [p.ps("A%d" % i, [128, 512], F32) for i in range(2)]
    B = [p.ps("B%d" % i, [128, 512], F32) for i in range(2)]
    Cn = [p.ps("Cn%d" % i, [128, 512], F32) for i in range(2)]
    pTk = p.ps("pTk", [128, 1024], BF16)
    pTh = p.ps("pTh", [128, 1024], BF16)

    gt = p.sb("gt", [128, NCH * 4], F32)
    p.dma(gt.v(gt.h[:, :].rearrange("p (j c) -> p j c", c=4)),
          gtm.v(gtm.h[:, :].rearrange("(j p) c -> p j c", p=128)))
    gt3 = gt.h[:, :].rearrange("p (j c) -> p j c", c=4)
    icb = p.sb("icb", [128, NCH * 2], F32)
    sp = p.sb("sp", [128, NCH * 2], F32)
    v3 = lambda t: t.h[:, :].rearrange("p (j c) -> p j c", c=2)
    p.tt(icb.v(v3(icb)), gt.v(gt3[:, :, 0:2]), ibf.v(v3(ibf)), ALU.add)
    p.tt(sp.v(v3(sp)), gt.v(gt3[:, :, 2:4]), fbf.v(v3(fbf)), ALU.add)
    if dbg == -1:
        return
    p.act(sp[:, :], sp[:, :], AF.Exp, scale=-1.0)
    p.act(sp[:, :], sp[:, :], AF.Ln, bias=1.0)
    NC2 = NCH * 2
    if dbg == -2:
        return
    p.mm(A[0][:, 0:NC2], tri[:, :], sp[:, :])
    p.mm(A[1][:, 0:NC2], ones[:, :], sp[:, :])
    eb = p.sb("eb", [128, NC2], F32)
    eu = p.sb("eu", [128, NC2], F32)
    ebl = p.sb("ebl", [128, NC2], F32)
    if dbg == -3:
        return
    p.act(eb[:, :], A[0][:, 0:NC2], AF.Exp, scale=-1.0)
    if dbg == -4:
        return
    p.tt(eu[:, :], icb[:, :], A[0][:, 0:NC2], ALU.add)
    if dbg == -5:
        return
    p.act(eu[:, :], eu[:, :], AF.Exp)
    if dbg == -6:
        return
    p.act(ebl[:, :], A[1][:, 0:NC2], AF.Exp, scale=-1.0)
    if dbg == -7:
        return
    eub = p.sb("eub", [128, NC2], BF16)
    p.copy(eub[:, :], eu[:, :])

    if dbg == 0:
        return
    qk = p.sb("qk", [128, 4, T_], BF16)
    xin = p.sb("xin", [128, T_ + 3], F32)
    acc = p.sb("acc", [128, T_], F32)
    p.memset(xin[:, 0:3], 0.0)
    HT = T_ // 2
    for c in range(min(4, dbg)):
        p.dma([xin[:, 3 + i * 2048:3 + (i + 1) * 2048] for i in range(4)],
              [qkfm[c * 128:(c + 1) * 128, i * 2048:(i + 1) * 2048] for i in range(4)])
        for hf in range(2):
            o0 = hf * HT
            e_ = "dve"
            p.ts(acc[:, o0:o0 + HT], xin[:, 3 + o0:3 + o0 + HT], cw[:, c * 4 + 3:c * 4 + 4], None, ALU.mult, eng=e_)
            for j in range(3):
                p.stt(acc[:, o0:o0 + HT], xin[:, j + o0:j + o0 + HT], cw[:, c * 4 + j:c * 4 + j + 1],
                      acc[:, o0:o0 + HT], ALU.mult, ALU.add, eng=e_)
        if c < 2:
            p.act(qk[:, c, :], acc[:, :], AF.Silu, bias=cb[:, c:c + 1])
        else:
            p.act(acc[:, :], acc[:, :], AF.Silu, bias=cb[:, c:c + 1])
            p.ts(qk[:, c, :], acc[:, :], 128.0 ** -0.5, None, ALU.mult)

    CN = [p.sb("CN%d" % i, [128, 257], F32) for i in range(2)]
    CNs = [p.sb("CNs%d" % i, [128, 257], F32) for i in range(2)]
    CNb = [p.sb("CNb%d" % i, [128, 257], BF16) for i in range(2)]
    for i in range(2):
        p.memset(CN[i][:, :], 0.0)
        p.memset(CNb[i][:, :], 0.0)
    vraw = [p.sb("vraw%d" % i, [128, 512], BF16) for i in range(3)]
    ogt = [p.sb("ogt%d" % i, [128, 512], F32) for i in range(3)]
    vp = [p.sb("vp%d" % i, [128, 257], BF16) for i in range(4)]
    PT = [p.sb("PT%d" % i, [128, 128], BF16) for i in range(4)]
    ktm = [p.sb("ktm%d" % i, [128, 128], BF16) for i in range(4)]
    dn = [p.sb("dn%d" % i, [128, 1], F32) for i in range(4)]
    dn2 = [p.sb("dnb%d" % i, [128, 1], F32) for i in range(4)]
    hbf = [p.sb("hbf%d" % i, [128, 256], BF16) for i in range(4)]
    hTs = [p.sb("hTs%d" % i, [128, 256], BF16) for i in range(4)]
    k = 0
    for j in range(nch):
        cs = slice(j * 128, (j + 1) * 128)
        vr = vraw[j % 3]
        og = ogt[j % 3]
        p.dma(vr[:, :], vtm[cs, :])
        p.dma(og[:, :], ogtm[cs, :])
        p.act(og[:, :], og[:, :], AF.Exp, scale=-1.0)
        p.ts(og[:, :], og[:, :], 1.0, None, ALU.add)
        p.recip(og[:, :], og[:, :])
        for hd in range(2):
            col = j * 2 + hd
            b4 = k % 4
            k += 1
            qT = qk[:, hd, cs]
            kT = qk[:, 2 + hd, cs]
            p.mm(A[hd][:, 0:128], kT, qT)
            p.tt(PT[b4][:, :], A[hd][:, 0:128], tri[:, :], ALU.mult)
            p.tr(pTk[:, hd * 128:(hd + 1) * 128], kT, ident[:, :])
            p.copy(ktm[b4][:, :], pTk[:, hd * 128:(hd + 1) * 128], eng="act")
            p.act(vp[b4][:, 0:256], vr[:, hd * 256:(hd + 1) * 256], AF.Copy, scale=eu[:, col:col + 1])
            p.copy(vp[b4][:, 256:257], eub[:, col:col + 1])
            p.mm(B[hd][:, 0:257], qT, CNb[hd][:, :], start=True, stop=False)
            p.mm(B[hd][:, 0:257], PT[b4][:, :], vp[b4][:, :], start=False, stop=True)
            p.mm(Cn[hd][:, 0:257], ktm[b4][:, :], vp[b4][:, :])
            if j == 0:
                p.copy(CNs[hd][:, :], Cn[hd][:, 0:257])
            else:
                pc = col - 2
                p.stt(CNs[hd][:, :], CNs[hd][:, :], ebl[:, pc:pc + 1], Cn[hd][:, 0:257], ALU.mult, ALU.add)
            p.act(CNb[hd][:, :], CNs[hd][:, :], AF.Copy, scale=ebl[:, col:col + 1])
            d_ = dn[b4]
            p.tt(d_[:, :], B[hd][:, 256:257], eb[:, col:col + 1], ALU.mult)
            p.stt(dn2[b4][:, :], d_[:, :], -1.0, d_[:, :], ALU.mult, ALU.max)
            p.ts(d_[:, :], dn2[b4][:, :], 1.0, None, ALU.max)
            p.recip(d_[:, :], d_[:, :])
            p.tt(d_[:, :], d_[:, :], eb[:, col:col + 1], ALU.mult)
            p.stt(hbf[b4][:, :], B[hd][:, 0:256], d_[:, 0:1], og[:, hd * 256:(hd + 1) * 256], ALU.mult, ALU.mult)
            for c in range(2):
                p.tr(pTh[:, hd * 256 + c * 128:hd * 256 + (c + 1) * 128], hbf[b4][:, c * 128:(c + 1) * 128], ident[:, :])
            p.copy(hTs[b4][:, :], pTh[:, hd * 256:(hd + 1) * 256], eng="act")
            r0 = (j // 32) * 512 + hd * 256
            cc = (j % 32) * 128
            p.dma([xi[r0 + c * 128:r0 + (c + 1) * 128, cc:cc + 128] for c in range(2)],
                  [hTs[b4][:, c * 128:(c + 1) * 128] for c in range(2)])


def c_perm():
    fmc = []
    for hp in range(2):
        for base in (0, 512):
            for hd in range(2):
                h = 2 * hp + hd
                fmc += list(range(base + h * 128, base + (h + 1) * 128))
    vcols = list(range(1024, 2048))
    ogcols = list(range(2048, 3072))
    gcols = list(range(3072, 3080))
    return fmc, vcols, ogcols, gcols


def mlstm_inputs(hp, qkfm, vtm, ogtm, gtm, conv_w, conv_b, ib, fb, consts):
    d = dict(consts)
    d["qkfm"] = None if qkfm is None else np.ascontiguousarray(qkfm)
    d["vtm"] = None if vtm is None else np.ascontiguousarray(vtm)
    d["ogtm"] = None if ogtm is None else np.ascontiguousarray(ogtm)
    d["gtm"] = None if gtm is None else np.ascontiguousarray(gtm)
    fmc, _, _, _ = c_perm()
    ch = np.asarray(fmc[hp * 512:(hp + 1) * 512]).reshape(4, 128)
    cwv = np.asarray(conv_w, np.float32)[:, ch]
    d["cw"] = np.ascontiguousarray(cwv.transpose(2, 1, 0).reshape(128, 16))
    d["cb"] = np.ascontiguousarray(np.asarray(conv_b, np.float32)[ch].T)
    nchk = SEQ // 128
    d["ibf"] = np.ascontiguousarray(np.tile(np.asarray(ib[2 * hp:2 * hp + 2], np.float32)[None, :], (128, nchk)))
    d["fbf"] = np.ascontiguousarray(np.tile(np.asarray(fb[2 * hp:2 * hp + 2], np.float32)[None, :], (128, nchk)))
    return d


I32 = mybir.dt.int32
NTC = BATCH * SEQ // NCORES
MLSTM_CONST_SPECS = (("tri", [128, 128], F32), ("ones", [128, 128], F32), ("cw", [128, 16], F32),
                     ("cb", [128, 4], F32), ("ibf", [128, 128], F32), ("fbf", [128, 128], F32))
ATTN_IN_SPECS = (("gbias", [128, NB * 12], F32), ("sinks", [128, 4], F32), ("posk", [64, 32], F32),
                 ("posv", [64, 32], F32), ("w1k", [64, 2048], F32), ("w1v", [64, 2048], F32),
                 ("w2k", [64, 64], F32), ("w2v", [64, 64], F32))


def build_fused():
    p = Prog()
    ext = lambda nm, shp, dt: p.dram(nm, shp, dt, "ExternalInput")
    scr = lambda nm, shp, dt: p.dram(nm, shp, dt, "Internal")
    xb = ext("xb", [SEQ, D], F32)
    xh = ext("xh", [NTC, D], F32)
    W0 = ext("W0", [D, 1036], F32)
    W1 = ext("W1", [D, 1540], F32)
    g0, gf0, g1, gf1, gfin = (ext(n, [128, 1024], F32) for n in ("g0", "gf0", "g1", "gf1", "gfin"))
    ai = {}
    for nm, shp, dt in ATTN_CONST_SPECS + ATTN_IN_SPECS:
        ai[nm] = ext(nm, shp, dt)
    ident = ai["ident"]
    mi = {"ident": ident}
    for nm, shp, dt in MLSTM_CONST_SPECS:
        mi[nm] = ext(nm, shp, dt)
    Wo0, Wo1 = ext("Wo0", [D, D], F32), ext("Wo1", [D, D], F32)
    Wg0, Wu0, Wg1, Wu1 = (ext(n, [D, FFN], F32) for n in ("Wg0", "Wu0", "Wg1", "Wu1"))
    Wd0, Wd1 = ext("Wd0", [FFN, D], F32), ext("Wd1", [FFN, D], F32)
    out = p.dram("out", [NTC, D], F32, "ExternalOutput")
    for t in (xb, xh):
        t.track = False
    s_fm = scr("s_fm", [832, SEQ], BF16)
    s_tmv = scr("s_tmv", [SEQ, 192], BF16)
    s_gts = scr("s_gts", [SEQ, 12], F32)
    xi2 = scr("xi2", [1024, NTC], BF16)
    s_h1 = scr("s_h1", [NTC, D], F32)
    s_h2 = scr("s_h2", [NTC, D], F32)
    xi3 = scr("xi3", [1024, NTC], BF16)
    s_qk = scr("s_qk", [512, SEQ], F32)
    s_v = scr("s_v", [SEQ, 512], BF16)
    s_og = scr("s_og", [SEQ, 512], F32)
    s_g = scr("s_g", [SEQ, 4], F32)
    xi4 = scr("xi4", [1024, NTC], BF16)
    s_h3 = scr("s_h3", [NTC, D], F32)
    for t in (s_fm, s_tmv, s_gts, xi2, s_h1, s_h2, xi3, s_qk, s_v, s_og, s_g, xi4, s_h3):
        t.track = False
    G2 = scr("G2", [NCORES * 1024, NTC], BF16)
    G3 = scr("G3", [NCORES * 1024, NTC], BF16)
    G4 = scr("G4", [NCORES * 1024, NTC], BF16)
    groups = [list(range(NCORES))]
    memo = {}

    def base_pairrows(e):
        k = ("a", p.emit_id)
        if k not in memo:
            pid = e.partition_id()
            memo[k] = e.snap((pid - pid % 2) * 1024 + (pid % 2) * 512, min_val=0, max_val=6 * 1024 + 512)
        return memo[k]

    def base_pair(e):
        k = ("b", p.emit_id)
        if k not in memo:
            pid = e.partition_id()
            memo[k] = e.snap((pid - pid % 2) * 1024, min_val=0, max_val=6 * 1024)
        return memo[k]

    def gather(Gt, xi):
        p.collective("AllGather", Gt.v(Gt.h.ap().opt()), xi.v(xi.h.ap().opt()), groups)

    p.begin_phase()
    phase_proj(p, SEQ, 832, BF16, [("v", 192, BF16), ("g", 12, F32)], xb, W0, g0, ident, s_fm, [s_tmv, s_gts])
    p.end_phase()
    p.begin_phase()
    phase_attn(p, s_fm, s_tmv, s_gts, ai, xi2)
    p.end_phase()
    p.begin_phase()
    gather(G2, xi2)
    phase_outproj(p, NTC, xh, G2, base_pairrows, Wo0, s_h1)
    p.end_phase()
    p.begin_phase()
    phase_ffn(p, NTC, s_h1, Wg0, Wu0, Wd0, gf0, ident, s_h2, nxt=(g1, xi3))
    p.end_phase()
    p.begin_phase()
    gather(G3, xi3)
    phase_proj(p, SEQ, 512, F32, [("v", 512, BF16), ("og", 512, F32), ("g", 4, F32)], None, W1, None, None,
               s_qk, [s_v, s_og, s_g], pre=(G3, base_pair))
    p.end_phase()
    p.begin_phase()
    phase_mlstm(p, s_qk, s_v, s_og, s_g, mi, xi4)
    p.end_phase()
    p.begin_phase()
    gather(G4, xi4)
    phase_outproj(p, NTC, s_h2, G4, base_pairrows, Wo1, s_h3)
    p.end_phase()
    p.begin_phase()
    phase_ffn(p, NTC, s_h3, Wg1, Wu1, Wd1, gf1, ident, out, final=True, gfin_d=gfin)
    return p


def kernel(x, norm_mix, norm_ffn, ffn_w_gate, ffn_w_up, ffn_w_down, ab_w_in, ab_gate_bias, nsa_pos_k, nsa_pos_v,
           nsa_cmp_k_w1, nsa_cmp_k_w2, nsa_cmp_v_w1, nsa_cmp_v_w2, swa_sinks, ab_w_out, c_w_in, c_conv_w, c_conv_b,
           c_igate_bias, c_fgate_bias, c_w_out, final_norm):
    f32 = lambda a: np.ascontiguousarray(np.asarray(a, np.float32))
    x = f32(x)
    fmc, tmc, gc = ab_perm()
    fmc1, vcols, ogcols, gcols = c_perm()
    aconst = attn_consts()
    mconst = mlstm_consts()
    common = dict(
        g0=g_rep_of(norm_mix[0]), gf0=g_rep_of(norm_ffn[0]), g1=g_rep_of(norm_mix[1]), gf1=g_rep_of(norm_ffn[1]),
        gfin=np.ascontiguousarray(np.broadcast_to(np.asarray(final_norm, np.float32)[None, :], (128, D))),
        Wo0=f32(np.asarray(ab_w_out[0])[list(range(0, 256)) + list(range(512, 768)) + list(range(256, 512))
                                          + list(range(768, 1024))]),
        Wo1=f32(c_w_out[0]),
        Wg0=f32(ffn_w_gate[0]), Wu0=f32(ffn_w_up[0]), Wd0=f32(ffn_w_down[0]),
        Wg1=f32(ffn_w_gate[1]), Wu1=f32(ffn_w_up[1]), Wd1=f32(ffn_w_down[1]),
    )
    in_maps = []
    for c in range(NCORES):
        b, g = c // 2, c % 2
        d = dict(common)
        d["xb"] = np.ascontiguousarray(x[b])
        d["xh"] = np.ascontiguousarray(x[b, g * NTC:(g + 1) * NTC])
        cols0 = fmc[g * 832:(g + 1) * 832] + tmc[g * 192:(g + 1) * 192] + gc[g * 12:(g + 1) * 12]
        d["W0"] = f32(np.asarray(ab_w_in[0])[:, cols0])
        a_in = attn_inputs(g, None, None, None, np.asarray(ab_gate_bias[0]), np.asarray(swa_sinks[0]),
                           nsa_pos_k[0], nsa_pos_v[0], nsa_cmp_k_w1[0], nsa_cmp_k_w2[0],
                           nsa_cmp_v_w1[0], nsa_cmp_v_w2[0], aconst)
        for k in ("fm", "tmv", "gts"):
            a_in.pop(k)
        d.update(a_in)
        gsel = [gcols[2 * g], gcols[2 * g + 1], gcols[4 + 2 * g], gcols[4 + 2 * g + 1]]
        cols1 = fmc1[g * 512:(g + 1) * 512] + vcols[g * 512:(g + 1) * 512] + ogcols[g * 512:(g + 1) * 512] + gsel
        d["W1"] = f32(np.asarray(c_w_in[0])[:, cols1])
        m_in = mlstm_inputs(g, None, None, None, None, c_conv_w[0], c_conv_b[0],
                            np.asarray(c_igate_bias[0]), np.asarray(c_fgate_bias[0]), mconst)
        for k in ("qkfm", "vtm", "ogtm", "gtm", "ident"):
            m_in.pop(k)
        d.update(m_in)
        in_maps.append(d)
    p = build_fused()
    res = run_prog(p, in_maps)
    out = np.concatenate([np.asarray(res[c]["out"]) for c in range(NCORES)], axis=0)
    return out.reshape(BATCH, SEQ, D).astype(np.float32)
```
